# Optimizing a Trainium2 kernel written in Bass

```python
import math
import jax, jax.numpy as jnp
from jax import lax
import numpy as np

D_MODEL = 2048
BATCH = 2
SEQ = 4096
DEPTH = 1

MIX_WIDTH = D_MODEL
ATTN_WIDTH = MIX_WIDTH // 2
RWKV_WIDTH = MIX_WIDTH - ATTN_WIDTH
HEAD_DIM = 64
ATTN_HEADS = ATTN_WIDTH // HEAD_DIM
ATTN_KV_HEADS = max(1, ATTN_HEADS // 8)
ATTN_GROUP = ATTN_HEADS // ATTN_KV_HEADS
WINDOW = 128
BLOCK = WINDOW
ROPE_THETA = 10000.0
RWKV_HEAD = 64
RWKV_HEADS = RWKV_WIDTH // RWKV_HEAD
DECAY_LORA = 64
A_LORA = 64
GATE_LORA = 160
RWKV_LN_EPS = 64e-5
Q_COLS = ATTN_HEADS * HEAD_DIM
KV_COLS = ATTN_KV_HEADS * HEAD_DIM
ATTN_COLS = Q_COLS + 2 * KV_COLS
RWKV_COLS = 3 * RWKV_WIDTH + DECAY_LORA + A_LORA + GATE_LORA
IN_COLS = ATTN_COLS + RWKV_COLS
N_GROUPS = 8
EXPERTS_PER_GROUP = 8
N_EXPERTS = N_GROUPS * EXPERTS_PER_GROUP
TOP_K = 2
EXPERT_FF = 512
MOE_BLOCK = 128
NORM_EPS = 1e-6

kernel_name = "hymba_swa_rwkv7_hmoe_adaln"

F32 = jnp.float32


def rms_norm(x, g):
    x32 = x.astype(F32)
    y = x32 * lax.rsqrt(jnp.mean(x32 * x32, axis=-1, keepdims=True) + NORM_EPS)
    return (y * g.astype(F32)).astype(x.dtype)


def modulate(h, shift, scale):
    return h * (1 + scale[:, None, :]) + shift[:, None, :]


def rope_tables(seq_len):
    inv_freq = ROPE_THETA ** (-jnp.arange(0, HEAD_DIM, 2, dtype=F32) / HEAD_DIM)
    ang = jnp.arange(seq_len, dtype=F32)[:, None] * inv_freq[None, :]
    return jnp.cos(ang), jnp.sin(ang)


def apply_rope(t, cos, sin):
    t32 = t.astype(F32)
    t1, t2 = jnp.split(t32, 2, axis=-1)
    cs, sn = cos[None, :, None, :], sin[None, :, None, :]
    return jnp.concatenate([t1 * cs - t2 * sn, t2 * cs + t1 * sn], axis=-1).astype(t.dtype)


def sliding_window_attention(q, k, v, sinks):
    B, T, H, Dh = q.shape
    nb = T // BLOCK
    qb = q.reshape(B, nb, BLOCK, ATTN_KV_HEADS, ATTN_GROUP, Dh)
    pad = ((0, 0), (BLOCK, 0), (0, 0), (0, 0))
    kp = jnp.pad(k, pad).reshape(B, nb + 1, BLOCK, ATTN_KV_HEADS, Dh)
    vp = jnp.pad(v, pad).reshape(B, nb + 1, BLOCK, ATTN_KV_HEADS, Dh)
    kb = jnp.concatenate([kp[:, :-1], kp[:, 1:]], axis=2)
    vb = jnp.concatenate([vp[:, :-1], vp[:, 1:]], axis=2)
    s = jnp.einsum('bnqhgd,bnkhd->bnhgqk', qb, kb, preferred_element_type=F32) * (1.0 / math.sqrt(Dh))
    qpos = jnp.arange(BLOCK)[:, None]
    kpos = jnp.arange(2 * BLOCK)[None, :] - BLOCK
    diff = qpos - kpos
    band = (diff >= 0) & (diff < WINDOW)
    in_seq = (jnp.arange(nb)[:, None, None] * BLOCK + kpos[None]) >= 0
    valid = band[None] & in_seq
    s = jnp.where(valid[None, :, None, None], s, -jnp.inf)
    sink = sinks.astype(F32).reshape(1, 1, ATTN_KV_HEADS, ATTN_GROUP, 1, 1)
    m = jnp.maximum(jnp.max(s, axis=-1, keepdims=True), sink)
    p = jnp.exp(s - m)
    denom = jnp.sum(p, axis=-1, keepdims=True) + jnp.exp(sink - m)
    o = jnp.einsum('bnhgqk,bnkhd->bnqhgd', p / denom, vb.astype(F32))
    return o.reshape(B, T, H * Dh).astype(q.dtype)


def token_shift(z, mu):
    z_prev = jnp.pad(z, ((0, 0), (1, 0), (0, 0)))[:, :-1]
    return z + (z_prev - z) * mu


def rwkv7_time_mix(z, w0, w_decay_up, a0, w_a_up, w_g_up, k_k, k_a, r_k, ln_w, ln_b):
    B, T, _ = z.shape
    W, H, N = RWKV_WIDTH, RWKV_HEADS, RWKV_HEAD
    r, k, v, wd, ad, gd = jnp.split(
        z, [W, 2 * W, 3 * W, 3 * W + DECAY_LORA, 3 * W + DECAY_LORA + A_LORA], axis=-1)
    w = -jax.nn.softplus(-(w0 + jnp.tanh(wd) @ w_decay_up)) - 0.5
    decay = jnp.exp(-jnp.exp(w.astype(F32)))
    a = jax.nn.sigmoid(a0 + ad @ w_a_up)
    g = jax.nn.sigmoid(gd) @ w_g_up
    kk = (k * k_k).astype(F32).reshape(B, T, H, N)
    kk = kk / jnp.maximum(jnp.sqrt(jnp.sum(kk * kk, axis=-1, keepdims=True)), 1e-12)
    k = k * (1 + (a - 1) * k_a)

    def heads(t):
        return t.astype(F32).reshape(B, T, H, N)

    r_h, k_h, v_h, a_h = heads(r), heads(k), heads(v), heads(a)
    xs = tuple(jnp.moveaxis(t, 1, 0) for t in
               (r_h, decay.reshape(B, T, H, N), k_h, v_h, -kk, kk * a_h))

    def step(S, inp):
        r_t, w_t, k_t, v_t, aa_t, bb_t = inp
        sa = jnp.einsum('bhvk,bhk->bhv', S, aa_t)
        S = S * w_t[:, :, None, :] + sa[..., None] * bb_t[:, :, None, :] + v_t[..., None] * k_t[:, :, None, :]
        return S, jnp.einsum('bhvk,bhk->bhv', S, r_t)

    S0 = jnp.zeros((B, H, N, N), F32)
    _, ys = lax.scan(step, S0, xs)
    y = jnp.moveaxis(ys, 0, 1)
    mean = jnp.mean(y, axis=-1, keepdims=True)
    var = jnp.mean(jnp.square(y - mean), axis=-1, keepdims=True)
    y = (y - mean) * lax.rsqrt(var + RWKV_LN_EPS)
    y = y.reshape(B, T, W) * ln_w.astype(F32) + ln_b.astype(F32)
    bonus = jnp.sum(r_h * k_h * r_k.astype(F32), axis=-1, keepdims=True) * v_h
    y = (y + bonus.reshape(B, T, W)) * g.astype(F32)
    return y.astype(z.dtype)


def hierarchical_moe(h, w_rg, b_rg, w_re, b_re, w1, w3, w2):
    B, T, D = h.shape
    xt = h.reshape(-1, D)
    N = xt.shape[0]
    g_prob = jax.nn.softmax((xt @ w_rg).astype(F32) + b_rg.astype(F32), axis=-1)
    g_gate, g_idx = lax.top_k(g_prob, 1)
    e_logits = ((xt @ w_re).astype(F32) + b_re.astype(F32)).reshape(N, N_GROUPS, EXPERTS_PER_GROUP)
    e_logits = jnp.take_along_axis(e_logits, g_idx[:, :, None], axis=1)[:, 0]
    e_top, e_idx = lax.top_k(jax.nn.softmax(e_logits, axis=-1), TOP_K)
    e_top = e_top / jnp.sum(e_top, axis=-1, keepdims=True)
    weights = g_gate * e_top
    expert = g_idx * EXPERTS_PER_GROUP + e_idx

    A = N * TOP_K
    flat_e = expert.reshape(-1)
    flat_w = weights.reshape(-1)
    flat_tok = jnp.arange(A) // TOP_K
    order = jnp.argsort(flat_e)
    se, stok, sw = flat_e[order], flat_tok[order], flat_w[order]
    counts = jnp.bincount(flat_e, length=N_EXPERTS)
    starts = jnp.cumsum(counts) - counts
    pcounts = (counts + MOE_BLOCK - 1) // MOE_BLOCK * MOE_BLOCK
    pends = jnp.cumsum(pcounts)
    pstarts = pends - pcounts
    dest = pstarts[se] + jnp.arange(A) - starts[se]
    n_blocks = -(-A // MOE_BLOCK) + N_EXPERTS
    P = n_blocks * MOE_BLOCK
    buf_tok = jnp.zeros((P,), jnp.int32).at[dest].set(stok.astype(jnp.int32))
    buf_w = jnp.zeros((P,), F32).at[dest].set(sw)
    block_expert = jnp.minimum(
        jnp.searchsorted(pends, jnp.arange(n_blocks) * MOE_BLOCK, side='right'), N_EXPERTS - 1)
    xb = xt[buf_tok].reshape(n_blocks, MOE_BLOCK, D)

    def expert_block(args):
        xblk, e = args
        hid = jax.nn.silu(xblk @ w1[e]) * (xblk @ w3[e])
        return hid @ w2[e]

    yb = lax.map(expert_block, (xb, block_expert)).reshape(P, D)
    yb = yb * buf_w[:, None].astype(yb.dtype)
    out = jnp.zeros_like(xt).at[buf_tok].add(yb)
    return out.reshape(B, T, D)


def setup_inputs(seed: int = 0) -> dict:
    key = jax.random.key(seed)
    ks = jax.random.split(key, 32)
    L, D = DEPTH, D_MODEL

    def nrm(k, shape, scale):
        return jax.random.normal(k, shape, F32) * scale

    return {
        "x": nrm(ks[0], (BATCH, SEQ, D), 1.0),
        "c": nrm(ks[1], (BATCH, D), 1.0),
        "w_ada": nrm(ks[2], (L, D, 6 * D), 0.02),
        "b_ada": nrm(ks[3], (L, 6 * D), 0.02),
        "norm1_g": 1.0 + nrm(ks[4], (L, D), 0.02),
        "w_in": nrm(ks[5], (L, D, IN_COLS), D ** -0.5),
        "mu_shift": jax.random.uniform(ks[6], (L, RWKV_COLS), F32),
        "sinks": nrm(ks[7], (L, ATTN_HEADS), 0.5),
        "w0": nrm(ks[8], (L, RWKV_WIDTH), 0.5) - 0.5,
        "w_decay_up": nrm(ks[9], (L, DECAY_LORA, RWKV_WIDTH), 0.1),
        "a0": nrm(ks[10], (L, RWKV_WIDTH), 0.5),
        "w_a_up": nrm(ks[11], (L, A_LORA, RWKV_WIDTH), 0.5 * A_LORA ** -0.5),
        "w_g_up": nrm(ks[12], (L, GATE_LORA, RWKV_WIDTH), GATE_LORA ** -0.5),
        "k_k": 1.0 + nrm(ks[13], (L, RWKV_WIDTH), 0.1),
        "k_a": 1.0 + nrm(ks[14], (L, RWKV_WIDTH), 0.1),
        "r_k": nrm(ks[15], (L, RWKV_HEADS, RWKV_HEAD), 0.1),
        "ln_x_w": 1.0 + nrm(ks[16], (L, RWKV_WIDTH), 0.02),
        "ln_x_b": nrm(ks[17], (L, RWKV_WIDTH), 0.02),
        "w_o": nrm(ks[18], (L, MIX_WIDTH, D), MIX_WIDTH ** -0.5),
        "norm2_g": 1.0 + nrm(ks[19], (L, D), 0.02),
        "w_router_group": nrm(ks[20], (L, D, N_GROUPS), D ** -0.5),
        "b_router_group": nrm(ks[21], (L, N_GROUPS), 0.01),
        "w_router_expert": nrm(ks[22], (L, D, N_EXPERTS), D ** -0.5),
        "b_router_expert": nrm(ks[23], (L, N_EXPERTS), 0.01),
        "w1": nrm(ks[24], (L, N_EXPERTS, D, EXPERT_FF), D ** -0.5),
        "w3": nrm(ks[25], (L, N_EXPERTS, D, EXPERT_FF), D ** -0.5),
        "w2": nrm(ks[26], (L, N_EXPERTS, EXPERT_FF, D), EXPERT_FF ** -0.5),
        "final_g": 1.0 + nrm(ks[27], (D,), 0.02),
    }


def reference(x, c, w_ada, b_ada, norm1_g, w_in, mu_shift, sinks, w0, w_decay_up, a0, w_a_up,
              w_g_up, k_k, k_a, r_k, ln_x_w, ln_x_b, w_o, norm2_g, w_router_group, b_router_group,
              w_router_expert, b_router_expert, w1, w3, w2, final_g):
    B, T, _ = x.shape
    cos, sin = rope_tables(T)
    for l in range(DEPTH):
        mod = jax.nn.silu(c) @ w_ada[l] + b_ada[l]
        sh1, sc1, gt1, sh2, sc2, gt2 = jnp.split(mod, 6, axis=-1)

        h = modulate(rms_norm(x, norm1_g[l]), sh1, sc1)
        z = h @ w_in[l]
        z_attn, z_rwkv = z[..., :ATTN_COLS], z[..., ATTN_COLS:]
        q, k, v = jnp.split(z_attn, [Q_COLS, Q_COLS + KV_COLS], axis=-1)
        q = apply_rope(q.reshape(B, T, ATTN_HEADS, HEAD_DIM), cos, sin)
        k = apply_rope(k.reshape(B, T, ATTN_KV_HEADS, HEAD_DIM), cos, sin)
        v = v.reshape(B, T, ATTN_KV_HEADS, HEAD_DIM)
        o_attn = sliding_window_attention(q, k, v, sinks[l])
        o_rwkv = rwkv7_time_mix(token_shift(z_rwkv, mu_shift[l]), w0[l], w_decay_up[l], a0[l],
                                w_a_up[l], w_g_up[l], k_k[l], k_a[l], r_k[l],
                                ln_x_w[l], ln_x_b[l])
        mixed = jnp.concatenate([o_attn, o_rwkv], axis=-1) @ w_o[l]
        x = x + gt1[:, None, :] * mixed

        h = modulate(rms_norm(x, norm2_g[l]), sh2, sc2)
        x = x + gt2[:, None, :] * hierarchical_moe(h, w_router_group[l], b_router_group[l],
                                                   w_router_expert[l], b_router_expert[l],
                                                   w1[l], w3[l], w2[l])
    return rms_norm(x, final_g)
```

```python
import contextlib
import numpy as np
import concourse.bass as bass
import concourse.mybir as mybir
from concourse.bass_utils import run_bass_kernel_spmd

F32 = mybir.dt.float32
BF16 = mybir.dt.bfloat16
AF = mybir.ActivationFunctionType
ALU = mybir.AluOpType
AX = mybir.AxisListType

D = 2048
KC = 16
NTOK = 4096
OWN = 1024
NB = 256
NBLK = NTOK // NB
FIRST_OWN_BLK = (NTOK - OWN) // NB
ATT_COLS = 1280
RW = 1024
NE = 64
FF = 512
DEBUG = {}


class Sched:
    ENGS = ("pe", "act", "dve", "pool", "sp")

    def __init__(self, nc, n_dma_sems=40):
        self.nc = nc
        self.prog = {e: [] for e in self.ENGS}
        self.cnt = {e: 0 for e in self.ENGS}
        self.waited = {e: {} for e in self.ENGS}
        self.last_w = {}
        self.readers = {}
        self.n_dma_sems = n_dma_sems
        self.dma_n = 0
        self.dma_uses = [0] * n_dma_sems
        self.last_ticket = None

    def _need(self, eng, reads, writes, samesync):
        need = {}

        def add(t):
            if t is None:
                return
            sk, v = t
            if need.get(sk, 0) < v:
                need[sk] = v
        for b in reads:
            add(self.last_w.get(b))
        for b in writes:
            add(self.last_w.get(b))
            for t in self.readers.get(b, ()):
                add(t)
        for sk, v in need.items():
            if sk == eng and not samesync:
                continue
            if self.waited[eng].get(sk, 0) >= v:
                continue
            self.prog[eng].append(("wait", sk, v))
            self.waited[eng][sk] = v

    def _commit(self, ticket, reads, writes):
        for b in writes:
            self.last_w[b] = ticket
            self.readers[b] = []
        for b in reads:
            self.readers.setdefault(b, []).append(ticket)
        self.last_ticket = ticket

    def op(self, eng, fn, reads=(), writes=(), samesync=True):
        pr = [k for k in reads if isinstance(k, str) and k.startswith("pb") and k not in writes]
        if pr:
            writes = list(writes) + pr
        self._need(eng, reads, writes, samesync)
        self.cnt[eng] += 1
        t = (eng, self.cnt[eng])
        self.prog[eng].append(("ins", fn, eng, 1))
        self._commit(t, reads, writes)
        return t

    def dma(self, q, out, in_, reads=(), writes=()):
        self._need(q, reads, writes, True)
        i = self.dma_n % self.n_dma_sems
        self.dma_n += 1
        sk = ("d", i)
        prev = 16 * self.dma_uses[i]
        if prev and self.waited[q].get(sk, 0) < prev:
            self.prog[q].append(("wait", sk, prev))
            self.waited[q][sk] = prev
        self.dma_uses[i] += 1
        t = (sk, 16 * self.dma_uses[i])
        self.prog[q].append(("ins", lambda e, o=out, s=in_: e.dma_start(out=o, in_=s), sk, 16))
        self._commit(t, reads, writes)
        return t

    def wait_ticket(self, eng, t):
        sk, v = t
        if self.waited[eng].get(sk, 0) < v:
            self.prog[eng].append(("wait", sk, v))
            self.waited[eng][sk] = v

    def barrier(self):
        for e in self.ENGS:
            for d in self.ENGS:
                if d != e and self.cnt[d]:
                    self.wait_ticket(e, (d, self.cnt[d]))
            for i in range(self.n_dma_sems):
                if self.dma_uses[i]:
                    self.wait_ticket(e, (("d", i), 16 * self.dma_uses[i]))

    def emit(self, stack):
        nc = self.nc
        for d in self.ENGS:
            if d != "sp" and self.cnt[d]:
                self.wait_ticket("sp", (d, self.cnt[d]))
        for i in range(self.n_dma_sems):
            if self.dma_uses[i]:
                self.wait_ticket("sp", (("d", i), 16 * self.dma_uses[i]))
        semh = {}
        for e in self.ENGS:
            semh[e] = stack.enter_context(nc.semaphore("s_" + e))
        for i in range(self.n_dma_sems):
            semh[("d", i)] = stack.enter_context(nc.semaphore("s_d%d" % i))
        block = stack.enter_context(nc.Block())

        def run(name):
            def body(e):
                for ent in self.prog[name]:
                    if ent[0] == "wait":
                        e.wait_ge(semh[ent[1]], ent[2])
                    else:
                        ent[1](e).then_inc(semh[ent[2]], ent[3])
            return body
        block.tensor(run("pe"))
        block.scalar(run("act"))
        block.vector(run("dve"))
        block.gpsimd(run("pool"))
        block.sync(run("sp"))


def build(dbg=None, stop_after=None, first_blk=0):
    dbg = dbg or {}
    nc = bass.Bass("TRN2", target_bir_lowering=False)
    S = Sched(nc)
    stack = contextlib.ExitStack()

    declared = []

    def din(name, shape, need=True):
        if not need:
            return None
        declared.append(name)
        return nc.dram_tensor(name, list(shape), F32, kind="ExternalInput").ap()
    moe_on = stop_after is None

    xw = din("xw", [NTOK, D])
    tmask = din("tmask", [128, NTOK])
    c_fm = din("c_fm", [128, KC])
    w_ada = din("w_ada", [D, 6 * D], "_modT" not in dbg)
    modT_in = din("modT_in", [128, 96], "_modT" in dbg)
    b_ada_fm = din("b_ada_fm", [128, 96])
    g1_fm = din("g1_fm", [128, KC])
    g2_fm = din("g2_fm", [128, KC])
    w_in = din("w_in", [D, 4640])
    mu_fm = din("mu_fm", [128, 28])
    w_dec = din("w_dec", [64, RW])
    w_aup = din("w_aup", [64, RW])
    w_gup = din("w_gup", [160, RW])
    w0_fm = din("w0_fm", [128, 8])
    a0_fm = din("a0_fm", [128, 8])
    kk_fm = din("kk_fm", [128, 8])
    ka_fm = din("ka_fm", [128, 8])
    rk_fm = din("rk_fm", [128, 8])
    lnw_fm = din("lnw_fm", [128, 8])
    lnb_fm = din("lnb_fm", [128, 8])
    sinks_bc = din("sinks_bc", [128, 16])
    w_o = din("w_o", [D, D])
    w_r = din("w_r", [D, 72])
    b_r_bc = din("b_r_bc", [128, 72])
    w1 = din("w1", [NE, D, FF], moe_on)
    w3 = din("w3", [NE, D, FF], moe_on)
    w2 = din("w2", [NE, FF, D], moe_on)
    fg_bc = din("fg_bc", [128, D])
    ident_d = din("ident", [128, 128])
    blk1_d = din("blk1", [128, 128])
    perm_d = din("perm", [128, 128])
    cosT_d = din("cosT", [128, 5 * NB])
    sinT_d = din("sinT", [128, 5 * NB])
    amask_d = din("amask", [128, 2, 256])
    mask1_d = din("mask1", [64, 2, 256])
    maskl_d = din("maskl", [64, 2, 64])
    ident2_d = din("ident2", [64, 2, 64])
    reset_d = din("resetm", [128, NB])
    out_d = nc.dram_tensor("out", [OWN, D], F32, kind="ExternalOutput").ap()
    dbg_out = {}
    for k, shp in dbg.items():
        if k.startswith("_"):
            continue
        dbg_out[k] = nc.dram_tensor("dbg_" + k, list(shp), F32, kind="ExternalOutput").ap()

    def sb(name, shape, dt=F32):
        return stack.enter_context(nc.sbuf_tensor(name, list(shape), dt))[:]

    def ps(name, shape, dt=F32):
        return stack.enter_context(nc.psum_tensor(name, list(shape), dt))[:]

    x1 = sb("x1", [128, 8, D])
    arena = sb("arena", [128, 31000])
    cst = sb("cst", [128, 3700])
    _c = [0]

    def carve(n, parts=128):
        a = cst[0:parts, _c[0]:_c[0] + n]
        _c[0] += n
        assert _c[0] <= 3700
        return a
    ident = carve(128); blk1 = carve(128); perm = carve(128)
    modT = carve(96); gs1 = carve(16); gs2 = carve(16); silc = carve(16); cfm = carve(16)
    g1t = carve(16); g2t = carve(16); bada = carve(96)
    mu = carve(28); w0t = carve(8); a0t = carve(8); kkt = carve(8); kat = carve(8); omka = carve(8)
    rkt = carve(8); lnw = carve(8); lnb = carve(8); sinks = carve(16); brbc = carve(72)
    amask = carve(512); mask1 = carve(512); maskl = carve(128); ident2 = carve(128)
    resetm = carve(NB); carry = carve(32)
    small = carve(256)
    ones128 = carve(128)
    PB = [ps("pb%d" % i, [128, 512]) for i in range(8)]

    def load_const(dst, src, key):
        S.dma("sp", dst, src, writes=[key])
    consts = [(ident, ident_d, "ident"), (blk1, blk1_d, "blk1"), (perm, perm_d, "perm"),
              (cfm, c_fm, "cfm"), (g1t, g1_fm, "g1t"), (g2t, g2_fm, "g2t"), (bada, b_ada_fm, "bada"),
              (mu, mu_fm, "mu"), (w0t, w0_fm, "w0t"), (a0t, a0_fm, "a0t"), (kkt, kk_fm, "kkt"),
              (kat, ka_fm, "kat"), (rkt, rk_fm, "rkt"), (lnw, lnw_fm, "lnw"), (lnb, lnb_fm, "lnb"),
              (sinks, sinks_bc, "sinks"), (brbc, b_r_bc, "brbc"),
              (amask, amask_d.rearrange("p a b -> p (a b)"), "amask"),
              (mask1[0:64, :], mask1_d.rearrange("p a b -> p (a b)"), "mask1"),
              (maskl[0:64, :], maskl_d.rearrange("p a b -> p (a b)"), "maskl"),
              (ident2[0:64, :], ident2_d.rearrange("p a b -> p (a b)"), "ident2"),
              (resetm, reset_d, "resetm")]
    for dst, src, key in consts:
        load_const(dst, src, key)
    S.op("pool", lambda e: e.memset(ones128, 1.0), writes=["ones128"])
    S.op("pool", lambda e: e.memset(carry, 0.0), writes=["carry%d" % g for g in range(32)])
    S.op("dve", lambda e: e.tensor_scalar(out=omka, in0=kat, scalar1=-1.0, scalar2=1.0,
                                          op0=ALU.mult, op1=ALU.add),
         reads=["kat"], writes=["omka"])

    def dump(name, src_ap, reads):
        if name in dbg_out:
            S.dma("sp", dbg_out[name], src_ap, reads=reads)

    class _Stop(Exception):
        pass

    def stop_here(tag):
        if stop_after == tag:
            raise _Stop()
    host_mod = "_modT" in dbg
    S.op("act", lambda e: e.activation(out=silc, in_=cfm, func=AF.Silu), reads=["cfm"], writes=["silc"])
    wada_v = w_ada.rearrange("(kc p) n -> p kc n", p=128) if w_ada is not None else None
    ADA_W = 512
    adabuf = [arena[:, i * KC * ADA_W:(i + 1) * KC * ADA_W].rearrange("p (k n) -> p k n", k=KC) for i in range(2)]
    pmod = PB[0][:, 0:96]
    if host_mod:
        S.dma("sp", modT, modT_in, writes=["modT"])
    for g in range(0 if host_mod else 6 * D // ADA_W):
        buf = adabuf[g % 2]
        bk = "adabuf%d" % (g % 2)
        S.dma("sp", buf, wada_v[:, :, g * ADA_W:(g + 1) * ADA_W], writes=[bk])
        for j in range(ADA_W // 128):
            cb = g * (ADA_W // 128) + j
            for kc in range(KC):
                S.op("pe", lambda e, cb=cb, kc=kc, j=j, buf=buf: e.matmul(
                    pmod[:, cb:cb + 1], lhsT=buf[:, kc, j * 128:(j + 1) * 128], rhs=silc[:, kc:kc + 1],
                    start=(kc == 0), stop=(kc == KC - 1)),
                    reads=[bk, "silc"], writes=["pmod"], samesync=False)
    if not host_mod:
        S.op("dve", lambda e: e.tensor_tensor(out=modT, in0=pmod, in1=bada, op=ALU.add),
             reads=["pmod", "bada"], writes=["modT"])
    S.op("dve", lambda e: e.scalar_tensor_tensor(out=gs1, in0=modT[:, 16:32], scalar=1.0, in1=g1t,
                                                 op0=ALU.add, op1=ALU.mult),
         reads=["modT", "g1t"], writes=["gs1"])
    S.op("dve", lambda e: e.scalar_tensor_tensor(out=gs2, in0=modT[:, 64:80], scalar=1.0, in1=g2t,
                                                 op0=ALU.add, op1=ALU.mult),
         reads=["modT", "g2t"], writes=["gs2"])
    dump("modT", modT, ["modT"])

    pe_cfg = [None]

    def pe_sync(stat):
        cfg = (stat.base_partition(), stat.partition_size(), stat.free_size())
        if pe_cfg[0] is not None and cfg != pe_cfg[0] and S.cnt["pe"]:
            S.wait_ticket("pe", ("pe", S.cnt["pe"]))
        pe_cfg[0] = cfg

    def mm(out, lhsT, rhs, start, stop, reads, writes):
        pe_sync(lhsT)
        S.op("pe", lambda e: e.matmul(out, lhsT=lhsT, rhs=rhs, start=start, stop=stop),
             reads=reads, writes=writes, samesync=False)

    def tr(out, in_, idn, reads, writes):
        pe_sync(in_)
        S.op("pe", lambda e: e.transpose(out, in_, idn), reads=list(reads) + ["ident"], writes=writes, samesync=False)

    def act(out, in_, func, reads, writes, **kw):
        S.op("act", lambda e: e.activation(out=out, in_=in_, func=func, **kw), reads=reads, writes=writes)

    def tt(eng, out, in0, in1, op, reads, writes):
        S.op(eng, lambda e: e.tensor_tensor(out=out, in0=in0, in1=in1, op=op), reads=reads, writes=writes)

    def ts(eng, out, in0, s1, s2, op0, op1, reads, writes):
        if s2 is None:
            S.op(eng, lambda e: e.tensor_scalar(out=out, in0=in0, scalar1=s1, scalar2=None, op0=op0),
                 reads=reads, writes=writes)
        else:
            S.op(eng, lambda e: e.tensor_scalar(out=out, in0=in0, scalar1=s1, scalar2=s2, op0=op0, op1=op1),
                 reads=reads, writes=writes)

    def stt(out, in0, scalar, in1, op0, op1, reads, writes):
        S.op("dve", lambda e: e.scalar_tensor_tensor(out=out, in0=in0, scalar=scalar, in1=in1, op0=op0, op1=op1),
             reads=reads, writes=writes)

    def cp(eng, out, in_, reads, writes):
        if eng == "act":
            S.op("act", lambda e: e.copy(out=out, in_=in_), reads=reads, writes=writes)
        else:
            S.op(eng, lambda e: e.tensor_copy(out=out, in_=in_), reads=reads, writes=writes)

    GT1 = sb("GT1", [128, D])
    bcs = arena[:, 0:128]

    def make_bcast(dst, col0, key, bcs=bcs, bk="bcs"):
        for kc in range(KC):
            ts("dve", bcs, ones128, modT[:, col0 + kc:col0 + kc + 1], None, ALU.mult, None,
               ["modT", "ones128"], [bk])
            tr(PB[1][:, 0:128], bcs, ident, [bk], ["pb1"])
            cp("act", dst[:, kc * 128:(kc + 1) * 128], PB[1][:, 0:128], ["pb1"], [key])
    make_bcast(GT1, 32, "GT1")
    dump("GT1", GT1, ["GT1"])
    if stop_after == "ada":
        S.emit(stack)
        return nc, stack, declared

    _a = [0]

    def acarve(n, parts=128):
        a = arena[0:parts, _a[0]:_a[0] + n]
        _a[0] += n
        assert _a[0] <= 31000, _a[0]
        return a
    for e in ("pe", "act", "dve", "pool", "sp"):
        S.wait_ticket(e, S.last_w["GT1"])
        S.wait_ticket(e, S.last_w["modT"])
    hT = acarve(KC * NB).rearrange("p (k n) -> p k n", k=KC)
    xtb = acarve(D)
    xn = acarve(D)
    wts = [acarve(KC * 128).rearrange("p (k n) -> p k n", k=KC) for _ in range(2)]
    Zt = [acarve(NB + 1) for _ in range(2)]
    ZSl = [acarve(NB) for _ in range(4)]
    ZSr = [[acarve(NB) for _ in range(3)] for _ in range(2)]
    tmt = acarve(NB)
    lwt = [acarve(4 * 128).rearrange("p (a n) -> p a n", a=4) for _ in range(2)]
    P_ = {n: acarve(NB) for n in ("sig", "cum", "a", "g", "t0", "t1", "kkn", "k2", "kna",
                                  "epos", "eneg", "eC", "bonus", "Bt", "Kt", "Bh", "Kh")}
    AR = acarve(2 * NB).rearrange("p (a n) -> p a n", a=2)
    TM = acarve(512, 64); M1 = acarve(512, 64).rearrange("p (h n) -> p h n", h=2)
    Lm = acarve(128, 64).rearrange("p (h n) -> p h n", h=2)
    XLb = [acarve(256, 64).rearrange("p (h a n) -> p h a n", h=2, a=2) for _ in range(2)]
    Pb = [acarve(128, 64).rearrange("p (h n) -> p h n", h=2) for _ in range(2)]
    AW = acarve(256, 64).rearrange("p (h n) -> p h n", h=2)
    AUs = acarve(256, 64).rearrange("p (h n) -> p h n", h=2)
    PhiT = acarve(128, 64).rearrange("p (h n) -> p h n", h=2)
    OmT = acarve(128, 64).rearrange("p (h n) -> p h n", h=2)
    Psis = acarve(128, 64).rearrange("p (h n) -> p h n", h=2)
    Dg = acarve(64)
    Mst = [acarve(16 * 64, 64).rearrange("p (h n) -> p h n", h=16) for _ in range(2)]
    Ysb = acarve(512, 64).rearrange("p (c h n) -> p c h n", c=4, h=2)
    gn = acarve(64, 64)
    oT = acarve(NB)
    wo = [acarve(512) for _ in range(2)]
    qraw = P_["Bt"]; qrot = P_["Kt"]; qtmp = P_["Bh"]
    kT = acarve(2 * 384).rearrange("p (h n) -> p h n", h=2)
    Vz = acarve(3 * 4 * 128).rearrange("p (t v n) -> p t v n", t=3, v=4)
    cst_ = P_["epos"]; snt_ = P_["eneg"]
    Ssb = P_["Kh"]; PTs = P_["kna"].rearrange("p (t n) -> p t n", t=2)

    S.op("pool", lambda e: e.memset(Mst[0], 0.0), writes=["M0_%d" % i for i in range(8)])
    S.op("pool", lambda e: e.memset(Vz, 0.0), writes=["Vz"])
    S.op("pool", lambda e: e.memset(kT, 0.0), writes=["kT"])
    w_in_v = w_in.rearrange("(kc p) n -> p kc n", p=128)
    sm_ss = small[:, 0:1]; sm_rt = small[:, 1:2]; sm_rstd = small[:, 2:3]
    RWB = ATT_COLS
    wt_i = [0]
    ps_i = [0]
    IN_SLOTS = [(PB[0][:, 0:256], "pb0"), (PB[1][:, 0:256], "pb1")]

    def inproj(col0, ncols, dup=False):
        wt = wts[wt_i[0] % 2]; wk = "wt%d" % (wt_i[0] % 2); wt_i[0] += 1
        if dup:
            S.dma("sp", wt[:, :, 0:64], w_in_v[:, :, col0:col0 + 64], writes=[wk])
            S.dma("sp", wt[:, :, 64:128], w_in_v[:, :, col0:col0 + 64], writes=[wk])
            ncols = 128
        else:
            S.dma("sp", wt[:, :, 0:ncols], w_in_v[:, :, col0:col0 + ncols], writes=[wk])
        pp, pk = IN_SLOTS[ps_i[0] % 2]; ps_i[0] += 1
        for kc in range(KC):
            mm(pp[0:ncols, :], wt[:, kc, 0:ncols], hT[:, kc, :], kc == 0, kc == KC - 1, [wk, "hT"], [pk])
        return pp[0:ncols, :], pk

    zt_i = [0]

    def tshift(pp, pk, ncols, g, dst, dkey):
        z = Zt[zt_i[0] % 2][0:ncols, :]; zk = "Zt%d" % (zt_i[0] % 2); zt_i[0] += 1
        cp("dve", z[:, 0:1], carry[0:ncols, g:g + 1], ["carry%d" % g], [zk])
        cp("act", z[:, 1:NB + 1], pp, [pk], [zk])
        tt("dve", dst[0:ncols, :], z[:, 0:NB], z[:, 1:NB + 1], ALU.subtract, [zk], [dkey])
        stt(dst[0:ncols, :], dst[0:ncols, :], mu[0:ncols, g:g + 1], z[:, 1:NB + 1], ALU.mult, ALU.add,
            [dkey, zk, "mu"], [dkey])
        cp("pool", carry[0:ncols, g:g + 1], z[:, NB:NB + 1], [zk], ["carry%d" % g])

    def outproj(wc, oTap, okey, blk):
        for cc in range(4):
            w = wo[cc % 2]; wk = "wo%d" % (cc % 2)
            c0 = cc * 512
            S.dma("sp", w, w_o[wc * 128:(wc + 1) * 128, c0:c0 + 512], writes=[wk])
            tt("pool", w, w, GT1[:, c0:c0 + 512], ALU.mult, [wk, "GT1"], [wk])
            for t2 in range(2):
                tile_i = (blk - FIRST_OWN_BLK) * 2 + t2
                mm(PB[2][:, :], oTap[:, t2 * 128:(t2 + 1) * 128], w, True, True, [okey, wk], ["pb2"])
                tt("dve", x1[:, tile_i, c0:c0 + 512], PB[2][:, :], x1[:, tile_i, c0:c0 + 512], ALU.add,
                   ["pb2", "x1_%d" % tile_i], ["x1_%d" % tile_i])

    GAM = -0.6065306597126334

    try:
      for blk in range(first_blk, NBLK):
          own = blk >= FIRST_OWN_BLK
          for t2 in range(2):
              t0 = blk * NB + t2 * 128
              if own:
                  tile_i = (blk - FIRST_OWN_BLK) * 2 + t2
                  xt = x1[:, tile_i, :]; xk = "x1_%d" % tile_i
              else:
                  xt = xtb; xk = "xtb"
              S.dma("sp", xt, xw[t0:t0 + 128, :], writes=[xk])
              act(xn, xt, AF.Square, [xk], ["xn", "ss"], accum_out=sm_ss)
              act(sm_rt, sm_ss, AF.Sqrt, ["ss"], ["rt"], scale=1.0 / D, bias=1e-6)
              S.op("dve", lambda e: e.reciprocal(out=sm_rstd, in_=sm_rt), reads=["rt"], writes=["rstd"])
              ts("dve", xn, xt, sm_rstd, None, ALU.mult, None, [xk, "rstd"], ["xn"])
              for g4 in range(4):
                  pbk = 2
                  for j in range(4):
                      kc = g4 * 4 + j
                      tr(PB[pbk][:, j * 128:(j + 1) * 128], xn[:, kc * 128:(kc + 1) * 128], ident, ["xn"],
                         ["pb%d" % pbk])
                  for j in range(4):
                      kc = g4 * 4 + j
                      act(hT[:, kc, t2 * 128:(t2 + 1) * 128], PB[pbk][:, j * 128:(j + 1) * 128], AF.Identity,
                          ["pb%d" % pbk, "gs1", "modT"], ["hT"],
                          scale=gs1[:, kc:kc + 1], bias=modT[:, kc:kc + 1])
          if not own:
              S.dma("sp", tmt, tmask[:, blk * NB:(blk + 1) * NB], writes=["tmt"])
              tt("dve", hT, hT, tmt.unsqueeze(1).to_broadcast([128, KC, NB]), ALU.mult, ["hT", "tmt"], ["hT"])
          if blk == FIRST_OWN_BLK:
              dump("hT12", hT.rearrange("p k n -> p (k n)"), ["hT"])
          stop_here("hT")

          if blk >= FIRST_OWN_BLK - 1:
              ab = blk - (FIRST_OWN_BLK - 1)
              S.dma("sp", cst_, cosT_d[:, ab * NB:(ab + 1) * NB], writes=["epos"])
              S.dma("sp", snt_, sinT_d[:, ab * NB:(ab + 1) * NB], writes=["eneg"])

              def rope(pp, pk, dst, dkey):
                  cp("act", qraw, pp, [pk], ["Bt"])
                  mm(PB[3][:, 0:256], perm, qraw, True, True, ["perm", "Bt"], ["pb3"])
                  tt("dve", qtmp, PB[3][:, 0:256], snt_, ALU.mult, ["pb3", "eneg"], ["Bh"])
                  tt("dve", dst, qraw, cst_, ALU.mult, ["Bt", "epos"], [dkey])
                  tt("dve", dst, dst, qtmp, ALU.add, [dkey, "Bh"], [dkey])
              for kvh in range(2):
                  pp, pk = inproj(1024 + kvh * 64, 64, dup=True)
                  rope(pp, pk, kT[:, kvh, 128:384], "kT")
              for t2 in range(2):
                  wt = wts[wt_i[0] % 2]; wk = "wt%d" % (wt_i[0] % 2); wt_i[0] += 1
                  if t2 == 0:
                      S.dma("sp", wt[:, :, 0:128], w_in_v[:, :, 1152:1280], writes=[wk])
                      wtv, wkv = wt, wk
                  else:
                      wt_i[0] -= 1
                  for kc in range(KC):
                      mm(PB[3][:, 256:384], hT[:, kc, t2 * 128:(t2 + 1) * 128], wtv[:, kc, 0:128], kc == 0, kc == KC - 1,
                         [wkv, "hT"], ["pb3"])
                  for kvh in range(2):
                      for par in range(2):
                          cp("act" if par else "dve", Vz[:, 1 + t2, kvh * 2 + par, par * 64:par * 64 + 64],
                             PB[3][:, 256 + kvh * 64:256 + kvh * 64 + 64], ["pb3"], ["Vz"])
              if own:
                  for hp in range(8):
                      kvh = hp // 4
                      pp, pk = inproj(hp * 128, 128)
                      rope(pp, pk, qrot, "Kt")
                      qM = (P_["sig"], P_["cum"])
                      ts("pool", qM[0], qrot, blk1[:, 0:1], None, ALU.mult, None, ["Kt", "blk1"], ["sig"])
                      ts("pool", qM[1], qrot, blk1[:, 64:65], None, ALU.mult, None, ["Kt", "blk1"], ["cum"])
                      for t2 in range(2):
                          qtile = (blk - FIRST_OWN_BLK) * 2 + t2
                          mi = 0 if qtile == 0 else 1
                          for par in range(2):
                              h = hp * 2 + par
                              b0 = par * 64
                              mm(PB[3][:, 256:512], qM[par][:, t2 * 128:(t2 + 1) * 128],
                                 kT[:, kvh, t2 * 128:t2 * 128 + 256], True, True, ["sig", "cum", "kT"], ["pb3"])
                              tt("dve", Ssb, PB[3][:, 256:512], amask[:, mi * 256:(mi + 1) * 256], ALU.add,
                                 ["pb3", "amask"], ["Kh"])
                              mx = small[:, 8:9]; nmx = small[:, 9:10]; rs = small[:, 10:11]; es = small[:, 11:12]
                              S.op("dve", lambda e, mx=mx: e.reduce_max(out=mx, in_=Ssb, axis=AX.X), reads=["Kh"], writes=["mx"])
                              ts("dve", nmx, mx, sinks[:, h:h + 1], -1.0, ALU.max, ALU.mult, ["mx", "sinks"], ["nmx"])
                              act(Ssb, Ssb, AF.Exp, ["Kh", "nmx"], ["Kh", "rs"], bias=nmx, accum_out=rs)
                              act(es, sinks[:, h:h + 1], AF.Exp, ["sinks", "nmx"], ["es"], bias=nmx)
                              tt("dve", rs, rs, es, ALU.add, ["rs", "es"], ["rs"])
                              S.op("dve", lambda e, rs=rs: e.reciprocal(out=rs, in_=rs), reads=["rs"], writes=["rs"])
                              ts("dve", Ssb, Ssb, rs, None, ALU.mult, None, ["Kh", "rs"], ["Kh"])
                              for kt in range(2):
                                  tr(PB[4][:, kt * 128:(kt + 1) * 128], Ssb[:, kt * 128:(kt + 1) * 128], ident, ["Kh"], ["pb4"])
                              cp("act", PTs.rearrange("p t n -> p (t n)"), PB[4][:, 0:256], ["pb4"], ["kna"])
                              for kt in range(2):
                                  mm(PB[5][:, t2 * 128:(t2 + 1) * 128], Vz[:, t2 + kt, kvh * 2 + par, :], PTs[:, kt, :],
                                     par == 0 and kt == 0, par == 1 and kt == 1, ["Vz", "kna"], ["pb5"])
                      cp("act", oT, PB[5][:, 0:256], ["pb5"], ["oT"])
                      if blk == FIRST_OWN_BLK and hp == 0:
                          dump("oTa", oT, ["oT"])
                      outproj(hp, oT, "oT", blk)
              cp("pool", kT[:, :, 0:128], kT[:, :, 256:384], ["kT"], ["kT"])
              cp("pool", Vz[:, 0, :, :], Vz[:, 2, :, :], ["Vz"], ["Vz"])
          stop_here("att")

          lora = [(RWB + 3072, 64, 24), (RWB + 3136, 64, 25), (RWB + 3200, 128, 26), (RWB + 3328, 32, 27)]
          for li, (c0, ncol, g) in enumerate(lora):
              pp, pk = inproj(c0, ncol)
              tshift(pp, pk, ncol, g, ZSl[li], "ZSl%d" % li)
          act(ZSl[0][0:64, :], ZSl[0][0:64, :], AF.Tanh, ["ZSl0"], ["ZSl0"])
          if own:
              act(ZSl[2], ZSl[2], AF.Sigmoid, ["ZSl2"], ["ZSl2"])
              act(ZSl[3][0:32, :], ZSl[3][0:32, :], AF.Sigmoid, ["ZSl3"], ["ZSl3"])
          stop_here("lora")
          for hp in range(8):
              zb = ZSr[hp % 2]; zbk = ["ZSr%d_%d" % (hp % 2, i) for i in range(3)]
              for i in range(3):
                  pp, pk = inproj(RWB + i * 1024 + hp * 128, 128)
                  tshift(pp, pk, 128, i * 8 + hp, zb[i], zbk[i])
              zr, zk_, zv = zb
              if blk == FIRST_OWN_BLK and hp == 0:
                  dump("zr", zr, [zbk[0]]); dump("zv", zv, [zbk[2]])
              lw = lwt[hp % 2]; lk = "lw%d" % (hp % 2)
              hs = slice(hp * 128, (hp + 1) * 128)
              S.dma("sp", lw[0:64, 0, :], w_dec[:, hs], writes=[lk])
              S.dma("sp", lw[0:64, 1, :], w_aup[:, hs], writes=[lk])
              if own:
                  S.dma("sp", lw[:, 2, :], w_gup[0:128, hs], writes=[lk])
                  S.dma("sp", lw[0:32, 3, :], w_gup[128:160, hs], writes=[lk])
              stop_here("rkv")
              p3a = PB[3][:, 0:256]; p3b = PB[3][:, 256:512]
              hcol = slice(hp, hp + 1)
              mm(p3a, lw[0:64, 0, :], ZSl[0][0:64, :], True, True, [lk, "ZSl0"], ["pb3"])
              act(P_["sig"], p3a, AF.Sigmoid, ["pb3", "w0t"], ["sig"], bias=w0t[:, hcol])
              ts("pool", P_["sig"], P_["sig"], GAM, None, ALU.mult, None, ["sig"], ["sig"])
              S.op("dve", lambda e: e.tensor_tensor_scan(out=P_["cum"], data0=resetm, data1=P_["sig"], initial=0.0,
                                                         op0=ALU.mult, op1=ALU.add),
                   reads=["sig", "resetm"], writes=["cum"])
              stop_here("decay")
              mm(p3b, lw[0:64, 1, :], ZSl[1][0:64, :], True, True, [lk, "ZSl1"], ["pb3"])
              act(P_["a"], p3b, AF.Sigmoid, ["pb3", "a0t"], ["a"], bias=a0t[:, hcol])
              ts("dve", P_["kkn"], zk_, kkt[:, hcol], None, ALU.mult, None, [zbk[1], "kkt"], ["kkn"])
              tt("pool", P_["t0"], P_["kkn"], P_["kkn"], ALU.mult, ["kkn"], ["t0"])
              mm(p3a, blk1, P_["t0"], True, True, ["blk1", "t0"], ["pb3"])
              act(P_["t1"], p3a, AF.Sqrt, ["pb3"], ["t1"], bias=1e-30)
              ts("dve", P_["t1"], P_["t1"], 1e-12, None, ALU.max, None, ["t1"], ["t1"])
              S.op("dve", lambda e: e.reciprocal(out=P_["t1"], in_=P_["t1"]), reads=["t1"], writes=["t1"])
              tt("dve", P_["kkn"], P_["kkn"], P_["t1"], ALU.mult, ["kkn", "t1"], ["kkn"])
              stop_here("kkn")
              ts("dve", P_["t0"], P_["a"], kat[:, hcol], omka[:, hcol], ALU.mult, ALU.add, ["a", "kat", "omka"], ["t0"])
              tt("dve", P_["k2"], zk_, P_["t0"], ALU.mult, [zbk[1], "t0"], ["k2"])
              tt("pool", P_["kna"], P_["kkn"], P_["a"], ALU.mult, ["kkn", "a"], ["kna"])
              stop_here("k2")
              act(P_["eneg"], P_["cum"], AF.Exp, ["cum"], ["eneg"], scale=-1.0)
              tt("dve", P_["t1"], P_["cum"], P_["sig"], ALU.subtract, ["cum", "sig"], ["t1"])
              act(P_["t1"], P_["t1"], AF.Exp, ["t1"], ["t1"])
              act(P_["epos"], P_["cum"], AF.Exp, ["cum"], ["epos"])
              for c in range(4):
                  cs_ = slice(c * 64, (c + 1) * 64)
                  act(P_["eC"][:, cs_], P_["cum"][:, cs_], AF.Exp, ["cum"], ["eC"], scale=-1.0,
                      bias=P_["cum"][:, c * 64 + 63:c * 64 + 64])
              stop_here("exps")
              stt(AR[:, 0, :], P_["kkn"], -1.0, P_["t1"], ALU.mult, ALU.mult, ["kkn", "t1"], ["AR"])
              tt("pool", P_["Bt"], P_["kna"], P_["eneg"], ALU.mult, ["kna", "eneg"], ["Bt"])
              tt("dve", P_["Kt"], P_["k2"], P_["eneg"], ALU.mult, ["k2", "eneg"], ["Kt"])
              tt("pool", P_["Bh"], P_["kna"], P_["eC"], ALU.mult, ["kna", "eC"], ["Bh"])
              tt("dve", P_["Kh"], P_["k2"], P_["eC"], ALU.mult, ["k2", "eC"], ["Kh"])
              stop_here("AR")
              if own:
                  tt("pool", AR[:, 1, :], zr, P_["epos"], ALU.mult, [zbk[0], "epos"], ["AR"])
                  mm(p3b, lw[:, 2, :], ZSl[2], True, False, [lk, "ZSl2"], ["pb3"])
                  mm(p3b, lw[0:32, 3, :], ZSl[3][0:32, :], False, True, [lk, "ZSl3"], ["pb3"])
                  cp("act", P_["g"], p3b, ["pb3"], ["g"])
                  stop_here("gate")
                  tt("dve", P_["t0"], zr, P_["k2"], ALU.mult, [zbk[0], "k2"], ["t0"])
                  ts("dve", P_["t0"], P_["t0"], rkt[:, hcol], None, ALU.mult, None, ["t0", "rkt"], ["t0"])
                  mm(p3a, blk1, P_["t0"], True, True, ["blk1", "t0"], ["pb3"])
                  tt("dve", P_["bonus"], p3a, zv, ALU.mult, ["pb3", zbk[2]], ["bonus"])
              BtM = (P_["sig"], P_["cum"]); KtM = (P_["a"], P_["t0"]); AtM = (P_["kkn"], P_["k2"])
              for h2 in range(2):
                  hmk = blk1[:, h2 * 64:h2 * 64 + 1]
                  ts("pool", BtM[h2], P_["Bt"], hmk, None, ALU.mult, None, ["Bt", "blk1"], [("sig", "cum")[h2]])
                  ts("dve", KtM[h2], P_["Kt"], hmk, None, ALU.mult, None, ["Kt", "blk1"], [("a", "t0")[h2]])
                  ts("pool", AtM[h2], AR[:, 0, :], hmk, None, ALU.mult, None, ["AR", "blk1"], [("kkn", "k2")[h2]])
              mm(PB[3][0:64, 0:4], ident[:, 64:128], P_["epos"].rearrange("p (c n) -> p c n", n=64)[:, :, 63],
                 True, True, ["ident", "epos"], ["pb3"])
              cp("act", Dg[0:64, 0:4], PB[3][0:64, 0:4], ["pb3"], ["Dg"])
              if own:
                  mm(PB[3][0:64, 256:512], ident[:, 64:128], AR[:, 1, :], True, True, ["ident", "AR"], ["pb3"])
                  cp("act", P_["t1"][0:64, :], PB[3][0:64, 256:512], ["pb3"], ["t1"])
              stop_here("prep")
              At = AR[:, 0, :]
              for c in range(4):
                  cs_ = slice(c * 64, (c + 1) * 64)
                  mcur = Mst[(blk * 4 + c) % 2]; mnew = Mst[(blk * 4 + c + 1) % 2]
                  mck = "M%d_%d" % ((blk * 4 + c) % 2, hp); mnk = "M%d_%d" % ((blk * 4 + c + 1) % 2, hp)
                  for i, (src, sk) in enumerate(((At, "AR"), (P_["Bh"], "Bh"), (P_["Kh"], "Kh"), (zv, zbk[2]))):
                      tr(PB[4][0:64, i * 128:(i + 1) * 128], src[:, cs_], ident, [sk], ["pb4"])
                  cp("act", TM[:, 128:512], PB[4][0:64, 128:512], ["pb4"], ["TM"])
                  cp("dve", AW[:, :, 0:64], PB[4][0:64, 0:128].rearrange("p (h n) -> p h n", h=2), ["pb4"], ["AW"])
                  stop_here("c_tm")
                  for h2 in range(2):
                      b0 = h2 * 64
                      nr = 2 if own else 1
                      mm(PB[5][0:64, h2 * 256:h2 * 256 + 64 * nr], BtM[h2][:, cs_], AR[:, 0:nr, cs_],
                         True, True, ["sig", "cum", "AR"], ["pb5"])
                      mm(PB[5][0:64, h2 * 256 + 128:h2 * 256 + 128 + 64 * nr], KtM[h2][:, cs_],
                         AR[:, 0:nr, cs_], True, True, ["a", "t0", "AR"], ["pb5"])
                      mm(PB[6][0:64, h2 * 64:(h2 + 1) * 64], AtM[h2][:, cs_], P_["Bt"][:, cs_], True, True,
                         ["kkn", "k2", "Bt"], ["pb6"])
                  if own:
                      tt("dve", M1.rearrange("p h n -> p (h n)"), PB[5][0:64, :], mask1[0:64, :], ALU.mult,
                         ["pb5", "mask1"], ["M1"])
                  else:
                      p5v = PB[5][0:64, :].rearrange("p (h n) -> p h n", h=2)
                      mkv = mask1[0:64, :].rearrange("p (h n) -> p h n", h=2)
                      for o_ in (0, 128):
                          tt("dve", M1[:, :, o_:o_ + 64], p5v[:, :, o_:o_ + 64], mkv[:, :, o_:o_ + 64], ALU.mult,
                             ["pb5", "mask1"], ["M1"])
                  tt("dve", Lm.rearrange("p h n -> p (h n)"), PB[6][0:64, 0:128], maskl[0:64, :], ALU.mult,
                     ["pb6", "maskl"], ["Lm"])
                  tt("pool", Pb[0], M1[:, :, 0:64], ident2[0:64, :].rearrange("p (h n) -> p h n", h=2), ALU.add,
                     ["M1", "ident2"], ["Pb0"])
                  stop_here("c_m1")
                  Xp = [M1[:, h2, 0:64] for h2 in range(2)]; Lp = [Lm[:, h2, :] for h2 in range(2)]
                  xk_, lk_ = ["M1"], ["Lm"]
                  pcur = 0
                  for m in range(5):
                      xl = XLb[m % 2]; xlk = "XL%d" % (m % 2)
                      for h2 in range(2):
                          mm(PB[6][0:64, 128 + h2 * 128:128 + h2 * 128 + 64], Lp[h2], Xp[h2], True, True, xk_ + lk_, ["pb6"])
                          mm(PB[6][0:64, 128 + h2 * 128 + 64:128 + h2 * 128 + 128], Xp[h2], Lp[h2], True, True, xk_ + lk_, ["pb6"])
                      cp("act", xl.rearrange("p h a n -> p (h a n)"), PB[6][0:64, 128:384], ["pb6"], [xlk])
                      Xp = [xl[:, h2, 0, :] for h2 in range(2)]; Lp = [xl[:, h2, 1, :] for h2 in range(2)]
                      xk_, lk_ = [xlk], [xlk]
                      for h2 in range(2):
                          mm(PB[6][0:64, 384 + h2 * 64:384 + (h2 + 1) * 64], Lp[h2], Pb[pcur][:, h2, :], True, True,
                             [xlk, "Pb%d" % pcur], ["pb6"])
                      tt("dve", Pb[1 - pcur].rearrange("p h n -> p (h n)"), PB[6][0:64, 384:512],
                         Pb[pcur].rearrange("p h n -> p (h n)"), ALU.add, ["pb6", "Pb%d" % pcur], ["Pb%d" % (1 - pcur)])
                      pcur = 1 - pcur
                  Pf = Pb[pcur]; pfk = "Pb%d" % pcur
                  stop_here("c_dbl")
                  for h2 in range(2):
                      mm(PB[6][0:64, h2 * 64:(h2 + 1) * 64], M1[:, h2, 128:192], TM[:, 384 + h2 * 64:384 + (h2 + 1) * 64],
                         True, True, ["M1", "TM"], ["pb6"])
                  cp("act", AW[:, :, 64:128], PB[6][0:64, 0:128].rearrange("p (h n) -> p h n", h=2), ["pb6"], ["AW"])
                  for h2 in range(2):
                      mm(PB[7][0:64, h2 * 128:(h2 + 1) * 128], Pf[:, h2, :], AW[:, h2, :], True, True, [pfk, "AW"], ["pb7"])
                  cp("act", AUs.rearrange("p h n -> p (h n)"), PB[7][0:64, 0:256], ["pb7"], ["AUs"])
                  stop_here("c_au")
                  for h2 in range(2):
                      b0 = h2 * 64
                      mm(PB[6][0:64, 128 + h2 * 64:128 + (h2 + 1) * 64], AUs[:, h2, 0:64], TM[:, 128 + b0:128 + b0 + 64],
                         True, True, ["AUs", "TM"], ["pb6"])
                      mm(PB[6][0:64, 256 + h2 * 64:256 + (h2 + 1) * 64], TM[:, 128 + b0:128 + b0 + 64], AUs[:, h2, 64:128],
                         True, False, ["AUs", "TM"], ["pb6"])
                      mm(PB[6][0:64, 256 + h2 * 64:256 + (h2 + 1) * 64], TM[:, 256 + b0:256 + b0 + 64],
                         TM[:, 384 + b0:384 + b0 + 64], False, True, ["TM"], ["pb6"])
                  stop_here("c_p1")
                  gcol = c * 64 + 63
                  stt(PhiT[:, 0, :], ident[0:64, 0:64], P_["epos"][0:64, gcol:gcol + 1], PB[6][0:64, 128:192],
                      ALU.mult, ALU.add, ["ident", "epos", "pb6"], ["PhiT"])
                  stt(PhiT[:, 1, :], ident[0:64, 0:64], Dg[0:64, c:c + 1], PB[6][0:64, 192:256],
                      ALU.mult, ALU.add, ["ident", "Dg", "pb6"], ["PhiT"])
                  cp("act", Psis.rearrange("p h n -> p (h n)"), PB[6][0:64, 256:384], ["pb6"], ["Psis"])
                  stop_here("c_phi")
                  if own:
                      for h2 in range(2):
                          mm(PB[6][0:64, 384 + h2 * 64:384 + (h2 + 1) * 64], AUs[:, h2, 0:64], M1[:, h2, 64:128],
                             True, True, ["AUs", "M1"], ["pb6"])
                      tt("dve", OmT[:, 0, :], PB[6][0:64, 384:448], AR[0:64, 1, cs_], ALU.add, ["pb6", "AR"], ["OmT"])
                      tt("dve", OmT[:, 1, :], PB[6][0:64, 448:512], P_["t1"][0:64, cs_], ALU.add, ["pb6", "t1"], ["OmT"])
                      for h2 in range(2):
                          hh = hp * 2 + h2
                          yo = PB[7][0:64, 256 + h2 * 64:256 + (h2 + 1) * 64]
                          mm(yo, OmT[:, h2, :], mcur[:, hh, :], True, False, ["OmT", mck], ["pb7"])
                          mm(yo, M1[:, h2, 64:128], AUs[:, h2, 64:128], False, False, ["M1", "AUs"], ["pb7"])
                          mm(yo, M1[:, h2, 192:256], TM[:, 384 + h2 * 64:384 + (h2 + 1) * 64], False, True, ["M1", "TM"], ["pb7"])
                      cp("act", Ysb[:, c, :, :], PB[7][0:64, 256:384].rearrange("p (h n) -> p h n", h=2), ["pb7"], ["Ysb"])
                  stop_here("c_y")
                  for h2 in range(2):
                      hh = hp * 2 + h2
                      mm(PB[7][0:64, 384 + h2 * 64:384 + (h2 + 1) * 64], PhiT[:, h2, :], mcur[:, hh, :], True, True,
                         ["PhiT", mck], ["pb7"])
                  tt("dve", mnew[:, hp * 2:hp * 2 + 2, :], PB[7][0:64, 384:512].rearrange("p (h n) -> p h n", h=2), Psis,
                     ALU.add, ["pb7", "Psis"], [mnk])
              stop_here("chunk")
              if own:
                  Yf = Ysb.rearrange("p c h n -> p (c h) n")
                  s1 = gn[:, 0:8]; s2 = gn[:, 8:16]
                  S.op("dve", lambda e: e.tensor_reduce(out=s1, in_=Yf, axis=AX.X, op=ALU.add), reads=["Ysb"], writes=["gn1"])
                  ts("dve", s1, s1, -1.0 / 64, None, ALU.mult, None, ["gn1"], ["gn1"])
                  tt("dve", Yf, Yf, s1.unsqueeze(2).to_broadcast([64, 8, 64]), ALU.add, ["Ysb", "gn1"], ["Ysb"])
                  ysq = TM[:, 0:512].rearrange("p (g n) -> p g n", g=8)
                  tt("dve", ysq, Yf, Yf, ALU.mult, ["Ysb"], ["TM"])
                  S.op("dve", lambda e: e.tensor_reduce(out=s2, in_=ysq, axis=AX.X, op=ALU.add), reads=["TM"], writes=["gn2"])
                  act(s2, s2, AF.Sqrt, ["gn2"], ["gn2"], scale=1.0 / 64, bias=64e-5)
                  S.op("dve", lambda e: e.reciprocal(out=s2, in_=s2), reads=["gn2"], writes=["gn2"])
                  tt("dve", Yf, Yf, s2.unsqueeze(2).to_broadcast([64, 8, 64]), ALU.mult, ["Ysb", "gn2"], ["Ysb"])
                  for c in range(4):
                      tr(PB[4][:, c * 64:(c + 1) * 64], Ysb[:, c, :, :].rearrange("p h n -> p (h n)"), ident[0:64, 0:64],
                         ["Ysb"], ["pb4"])
                  act(oT, PB[4][:, 0:256], AF.Identity, ["pb4", "lnw", "lnb"], ["oT"], scale=lnw[:, hcol], bias=lnb[:, hcol])
                  tt("dve", oT, oT, P_["bonus"], ALU.add, ["oT", "bonus"], ["oT"])
                  tt("dve", oT, oT, P_["g"], ALU.mult, ["oT", "g"], ["oT"])
                  if blk == FIRST_OWN_BLK and hp == 0:
                      dump("oTr", oT, ["oT"])
                  outproj(8 + hp, oT, "oT", blk)
    except _Stop:
        pass
    dump("x1", x1.rearrange("p t n -> p (t n)"), ["x1_%d" % i for i in range(8)])
    dump("Mst", Mst[0].rearrange("p h n -> p (h n)"), ["M0_%d" % i for i in range(8)])
    if stop_after is not None:
        S.emit(stack)
        return nc, stack, declared

    S.barrier()
    _a[0] = 0
    h2T = acarve(KC * OWN).rearrange("p (k n) -> p k n", k=KC)
    hid = acarve(4 * OWN).rearrange("p (f n) -> p f n", f=4)
    w13 = [[acarve(KC * 128).rearrange("p (k n) -> p k n", k=KC) for _ in range(2)] for _ in range(2)]
    w2t = acarve(4 * 256).rearrange("p (f n) -> p f n", f=4)
    Wt = acarve(8 * 64).rearrange("p (t e) -> p t e", t=8)
    rsc = acarve(256)
    bcs2 = acarve(128)
    xn2 = hid.rearrange("p f n -> p (f n)")[:, 0:D]
    wr = w13[0][0].rearrange("p k n -> p (k n)")[:, 0:KC * 72].rearrange("p (k n) -> p k n", k=KC)
    make_bcast(GT1, 80, "GT1", bcs2, "bcs2")
    GT2 = GT1
    S.dma("sp", wr, w_r.rearrange("(kc p) n -> p kc n", p=128), writes=["w13_0_0"])
    for ti in range(8):
        xt = x1[:, ti, :]; xk = "x1_%d" % ti
        act(xn2, xt, AF.Square, [xk], ["hid", "ss"], accum_out=sm_ss)
        act(sm_rt, sm_ss, AF.Sqrt, ["ss"], ["rt"], scale=1.0 / D, bias=1e-6)
        S.op("dve", lambda e: e.reciprocal(out=sm_rstd, in_=sm_rt), reads=["rt"], writes=["rstd"])
        ts("dve", xn2, xt, sm_rstd, None, ALU.mult, None, [xk, "rstd"], ["hid"])
        for g4 in range(4):
            for j in range(4):
                kc = g4 * 4 + j
                tr(PB[1][:, j * 128:(j + 1) * 128], xn2[:, kc * 128:(kc + 1) * 128], ident, ["hid"], ["pb1"])
            for j in range(4):
                kc = g4 * 4 + j
                act(h2T[:, kc, ti * 128:(ti + 1) * 128], PB[1][:, j * 128:(j + 1) * 128], AF.Identity,
                    ["pb1", "gs2", "modT"], ["h2T"], scale=gs2[:, kc:kc + 1], bias=modT[:, 48 + kc:49 + kc])
        for kc in range(KC):
            mm(PB[3][:, 0:72], h2T[:, kc, ti * 128:(ti + 1) * 128], wr[:, kc, :], kc == 0, kc == KC - 1,
               ["h2T", "w13_0_0"], ["pb3"])
        L = rsc[:, 0:72]; lg = rsc[:, 0:8]; le = rsc[:, 8:72]
        ohg = rsc[:, 72:80]; tmp64 = rsc[:, 80:144]; lsel = rsc[:, 144:152]; oh1 = rsc[:, 152:160]
        l2 = rsc[:, 160:168]; oh2 = rsc[:, 168:176]; we = rsc[:, 176:184]; eg = rsc[:, 184:192]
        mg = small[:, 16:17]; nmg = small[:, 17:18]; sg = small[:, 18:19]; m1 = small[:, 19:20]; m2 = small[:, 20:21]
        dd = small[:, 21:22]; e1 = small[:, 22:23]; e2 = small[:, 23:24]
        tt("dve", L, PB[3][:, 0:72], brbc, ALU.add, ["pb3", "brbc"], ["rsc"])
        S.op("dve", lambda e: e.reduce_max(out=mg, in_=lg, axis=AX.X), reads=["rsc"], writes=["rsm"])
        ts("dve", ohg, lg, mg, None, ALU.is_equal, None, ["rsc", "rsm"], ["rsc"])
        ts("dve", nmg, mg, -1.0, None, ALU.mult, None, ["rsm"], ["rsm"])
        act(eg, lg, AF.Exp, ["rsc", "rsm"], ["rsc", "rsm"], bias=nmg, accum_out=sg)
        S.op("dve", lambda e: e.reciprocal(out=sg, in_=sg), reads=["rsm"], writes=["rsm"])
        tt("dve", tmp64.rearrange("p (g e) -> p g e", g=8), le.rearrange("p (g e) -> p g e", g=8),
           ohg.unsqueeze(2).to_broadcast([128, 8, 8]), ALU.mult, ["rsc"], ["rsc"])
        S.op("dve", lambda e: e.tensor_reduce(out=lsel, in_=tmp64.rearrange("p (g e) -> p e g", g=8), axis=AX.X, op=ALU.add),
             reads=["rsc"], writes=["rsc"])
        S.op("dve", lambda e: e.reduce_max(out=m1, in_=lsel, axis=AX.X), reads=["rsc"], writes=["rsm"])
        ts("dve", oh1, lsel, m1, None, ALU.is_equal, None, ["rsc", "rsm"], ["rsc"])
        stt(l2, oh1, -1e30, lsel, ALU.mult, ALU.add, ["rsc"], ["rsc"])
        S.op("dve", lambda e: e.reduce_max(out=m2, in_=l2, axis=AX.X), reads=["rsc"], writes=["rsm"])
        ts("dve", oh2, l2, m2, None, ALU.is_equal, None, ["rsc", "rsm"], ["rsc"])
        tt("dve", dd, m1, m2, ALU.subtract, ["rsm"], ["rsm"])
        act(e1, dd, AF.Sigmoid, ["rsm"], ["rsm"])
        act(e2, dd, AF.Sigmoid, ["rsm"], ["rsm"], scale=-1.0)
        ts("dve", oh1, oh1, e1, None, ALU.mult, None, ["rsc", "rsm"], ["rsc"])
        stt(we, oh2, e2, oh1, ALU.mult, ALU.add, ["rsc", "rsm"], ["rsc"])
        ts("dve", we, we, sg, None, ALU.mult, None, ["rsc", "rsm"], ["rsc"])
        tt("dve", Wt[:, ti, :].rearrange("p (g e) -> p g e", g=8), ohg.unsqueeze(2).to_broadcast([128, 8, 8]),
           we.unsqueeze(1).to_broadcast([128, 8, 8]), ALU.mult, ["rsc"], ["Wt"])
    dump("Wt", Wt.rearrange("p t e -> p (t e)"), ["Wt"])
    dump("h2T", h2T.rearrange("p k n -> p (k n)"), ["h2T"])
    n_exp = dbg.get("_n_exp", NE)
    wi = [0]
    for ex in range(n_exp if moe_on else 0):
        for ffc in range(4):
            b = wi[0] % 2; wi[0] += 1
            w1c, w3c = w13[b]; k1, k3 = "w13_%d_0" % b, "w13_%d_1" % b
            S.dma("sp", w1c, w1[ex].rearrange("(kc p) n -> p kc n", p=128)[:, :, ffc * 128:(ffc + 1) * 128], writes=[k1])
            S.dma("sp", w3c, w3[ex].rearrange("(kc p) n -> p kc n", p=128)[:, :, ffc * 128:(ffc + 1) * 128], writes=[k3])
            for th in range(2):
                tsl = slice(th * 512, (th + 1) * 512)
                for kc in range(KC):
                    mm(PB[0][:, :], w1c[:, kc, :], h2T[:, kc, tsl], kc == 0, kc == KC - 1, [k1, "h2T"], ["pb0"])
                for kc in range(KC):
                    mm(PB[1][:, :], w3c[:, kc, :], h2T[:, kc, tsl], kc == 0, kc == KC - 1, [k3, "h2T"], ["pb1"])
                act(hid[:, ffc, tsl], PB[0][:, :], AF.Silu, ["pb0"], ["hid"])
                tt("dve", hid[:, ffc, tsl], hid[:, ffc, tsl], PB[1][:, :], ALU.mult, ["hid", "pb1"], ["hid"])
        for cq in range(8):
            csl = slice(cq * 256, (cq + 1) * 256)
            S.dma("sp", w2t, w2[ex].rearrange("(f p) n -> p f n", p=128)[:, :, csl], writes=["w2t"])
            tt("pool", w2t, w2t, GT2[:, csl].unsqueeze(1).to_broadcast([128, 4, 256]), ALU.mult, ["w2t", "GT1"], ["w2t"])
            for ti in range(8):
                slot = (cq * 8 + ti) % 4
                pp = PB[2 + slot][:, 0:256]; pk = "pb%d" % (2 + slot)
                for ffc in range(4):
                    mm(pp, hid[:, ffc, ti * 128:(ti + 1) * 128], w2t[:, ffc, :], ffc == 0, ffc == 3, ["hid", "w2t"], [pk])
                stt(x1[:, ti, csl], pp, Wt[:, ti, ex:ex + 1], x1[:, ti, csl], ALU.mult, ALU.add,
                    [pk, "Wt", "x1_%d" % ti], ["x1_%d" % ti])
    dump("x2", x1.rearrange("p t n -> p (t n)"), ["x1_%d" % i for i in range(8)])
    S.barrier()
    fg = h2T.rearrange("p k n -> p (k n)")[:, 0:D]
    ob = [h2T.rearrange("p k n -> p (k n)")[:, D * (1 + i):D * (2 + i)] for i in range(2)]
    jk = h2T.rearrange("p k n -> p (k n)")[:, 3 * D:4 * D]
    S.dma("sp", fg, fg_bc, writes=["fg"])
    outs = []
    for ti in range(8):
        xt = x1[:, ti, :]; xk = "x1_%d" % ti
        o = ob[ti % 2]; ok = "ob%d" % (ti % 2)
        act(jk, xt, AF.Square, [xk], ["jk", "ss"], accum_out=sm_ss)
        act(sm_rt, sm_ss, AF.Sqrt, ["ss"], ["rt"], scale=1.0 / D, bias=1e-6)
        S.op("dve", lambda e: e.reciprocal(out=sm_rstd, in_=sm_rt), reads=["rt"], writes=["rstd"])
        stt(o, xt, sm_rstd, fg, ALU.mult, ALU.mult, [xk, "rstd", "fg"], [ok])
        outs.append(S.dma("sp", out_d[ti * 128:(ti + 1) * 128, :], o, reads=[ok]))
    for t in outs:
        S.wait_ticket("sp", t)
    S.emit(stack)
    return nc, stack, declared


def _fm(v, n):
    return np.ascontiguousarray(np.asarray(v, np.float32).reshape(n, 128).T)


def _host_inputs(inp):
    f = np.float32
    x = np.asarray(inp["x"], f)
    c = np.asarray(inp["c"], f)
    w_in = np.ascontiguousarray(np.asarray(inp["w_in"], f)[0])
    shared = {}
    shared["w_ada"] = np.ascontiguousarray(np.asarray(inp["w_ada"], f)[0])
    shared["b_ada_fm"] = _fm(inp["b_ada"][0], 96)
    shared["g1_fm"] = _fm(inp["norm1_g"][0], 16)
    shared["g2_fm"] = _fm(inp["norm2_g"][0], 16)
    shared["w_in"] = w_in
    mu = np.asarray(inp["mu_shift"], f)[0]
    mu_fm = np.zeros((128, 28), f)
    for g in range(24):
        mu_fm[:, g] = mu[g * 128:(g + 1) * 128]
    mu_fm[0:64, 24] = mu[3072:3136]
    mu_fm[0:64, 25] = mu[3136:3200]
    mu_fm[:, 26] = mu[3200:3328]
    mu_fm[0:32, 27] = mu[3328:3360]
    shared["mu_fm"] = mu_fm
    shared["w_dec"] = np.ascontiguousarray(np.asarray(inp["w_decay_up"], f)[0])
    shared["w_aup"] = np.ascontiguousarray(np.asarray(inp["w_a_up"], f)[0])
    shared["w_gup"] = np.ascontiguousarray(np.asarray(inp["w_g_up"], f)[0])
    shared["w0_fm"] = _fm(inp["w0"][0], 8)
    shared["a0_fm"] = _fm(inp["a0"][0], 8)
    shared["kk_fm"] = _fm(inp["k_k"][0], 8)
    shared["ka_fm"] = _fm(inp["k_a"][0], 8)
    shared["rk_fm"] = _fm(np.asarray(inp["r_k"], f)[0].reshape(-1), 8)
    shared["lnw_fm"] = _fm(inp["ln_x_w"][0], 8)
    shared["lnb_fm"] = _fm(inp["ln_x_b"][0], 8)
    shared["sinks_bc"] = np.ascontiguousarray(np.broadcast_to(np.asarray(inp["sinks"], f)[0][None, :], (128, 16)))
    shared["w_o"] = np.ascontiguousarray(np.asarray(inp["w_o"], f)[0])
    shared["w_r"] = np.ascontiguousarray(np.concatenate(
        [np.asarray(inp["w_router_group"], f)[0], np.asarray(inp["w_router_expert"], f)[0]], axis=1))
    b_r = np.concatenate([np.asarray(inp["b_router_group"], f)[0], np.asarray(inp["b_router_expert"], f)[0]])
    shared["b_r_bc"] = np.ascontiguousarray(np.broadcast_to(b_r[None, :], (128, 72)))
    shared["w1"] = np.asarray(inp["w1"], f)[0]
    shared["w3"] = np.asarray(inp["w3"], f)[0]
    shared["w2"] = np.asarray(inp["w2"], f)[0]
    shared["fg_bc"] = np.ascontiguousarray(np.broadcast_to(np.asarray(inp["final_g"], f)[None, :], (128, D)))
    shared["ident"] = np.eye(128, dtype=f)
    b1 = np.zeros((128, 128), f); b1[:64, :64] = 1; b1[64:, 64:] = 1
    shared["blk1"] = b1
    pm = np.zeros((128, 128), f)
    for m in range(128):
        base = (m // 64) * 64; i = m % 64
        pm[base + (i + 32) % 64, m] = 1
    shared["perm"] = pm
    up_s = np.triu(np.ones((64, 64), f), 1); up_i = np.triu(np.ones((64, 64), f), 0)
    m1 = np.concatenate([up_s, up_i, up_s, up_i], axis=1)
    shared["mask1"] = np.ascontiguousarray(np.stack([m1, m1], axis=1))
    lo_s = np.tril(np.ones((64, 64), f), -1)
    shared["maskl"] = np.ascontiguousarray(np.stack([lo_s, lo_s], axis=1))
    shared["ident2"] = np.ascontiguousarray(np.stack([np.eye(64, dtype=f)] * 2, axis=1))
    rm = np.ones((128, NB), f); rm[:, ::64] = 0
    shared["resetm"] = rm
    inv_freq = (10000.0 ** (-np.arange(0, 64, 2, dtype=f) / f(64))).astype(f)
    qpos = np.arange(128)[:, None]; kpos = np.arange(256)[None, :] - 128
    diff = qpos - kpos
    band = (diff >= 0) & (diff < 128)
    per_core = []
    for core in range(8):
        b, q = core // 4, core % 4
        m = dict(shared)
        start = 1024 * (q + 1) - NTOK
        win = np.zeros((NTOK, D), f)
        lo = max(start, 0)
        win[lo - start:] = x[b, lo:1024 * (q + 1)]
        m["xw"] = win
        tm = np.zeros((128, NTOK), f); tm[:, lo - start:] = 1
        m["tmask"] = tm
        m["c_fm"] = _fm(c[b], 16)
        pos = (np.arange(5 * NB) + (1024 * q - NB)).astype(f)
        ang = pos[None, :] * np.tile(inv_freq, 4)[:, None]
        cs = np.cos(ang).astype(f) * f(0.125 ** 0.5)
        sn = np.sin(ang).astype(f) * f(0.125 ** 0.5)
        sign = np.where((np.arange(128) % 64) < 32, -1.0, 1.0).astype(f)[:, None]
        m["cosT"] = np.ascontiguousarray(cs)
        m["sinT"] = np.ascontiguousarray(sn * sign)
        am = np.where(band, 0.0, -30000.0).astype(f)
        am0 = am.copy()
        if q == 0:
            am0[:, :128] = -30000.0
        m["amask"] = np.ascontiguousarray(np.stack([am0, am], axis=1))
        per_core.append(m)
    return per_core


def kernel(**inputs):
    nc, stack, declared = build()
    with stack:
        pass
    in_maps = [{k: m[k] for k in declared} for m in _host_inputs(inputs)]
    res = run_bass_kernel_spmd(nc, in_maps, core_ids=list(range(8)))
    out = np.zeros((2, 4096, D), np.float32)
    for core in range(8):
        b, q = core // 4, core % 4
        out[b, 1024 * q:1024 * (q + 1)] = res.results[core]["out"]
    return out
```

```python
import contextlib
import numpy as np
import concourse.bass as bass
import concourse.mybir as mybir
from concourse.bass_utils import run_bass_kernel_spmd

F32 = mybir.dt.float32
BF16 = mybir.dt.bfloat16
AF = mybir.ActivationFunctionType
ALU = mybir.AluOpType
AX = mybir.AxisListType

D = 2048
KC = 16
NTOK = 4096
OWN = 1024
NB = 256
NBLK = NTOK // NB
FIRST_OWN_BLK = (NTOK - OWN) // NB
ATT_COLS = 1280
RW = 1024
NE = 64
FF = 512
DEBUG = {}


class Sched:
    ENGS = ("pe", "act", "dve", "pool", "sp")

    def __init__(self, nc, n_dma_sems=40):
        self.nc = nc
        self.prog = {e: [] for e in self.ENGS}
        self.cnt = {e: 0 for e in self.ENGS}
        self.waited = {e: {} for e in self.ENGS}
        self.last_w = {}
        self.readers = {}
        self.n_dma_sems = n_dma_sems
        self.dma_n = 0
        self.dma_uses = [0] * n_dma_sems
        self.last_ticket = None

    def _need(self, eng, reads, writes, samesync):
        need = {}

        def add(t):
            if t is None:
                return
            sk, v = t
            if need.get(sk, 0) < v:
                need[sk] = v
        for b in reads:
            add(self.last_w.get(b))
        for b in writes:
            add(self.last_w.get(b))
            for t in self.readers.get(b, ()):
                add(t)
        for sk, v in need.items():
            if sk == eng and not samesync:
                continue
            if self.waited[eng].get(sk, 0) >= v:
                continue
            self.prog[eng].append(("wait", sk, v))
            self.waited[eng][sk] = v

    def _commit(self, ticket, reads, writes):
        for b in writes:
            self.last_w[b] = ticket
            self.readers[b] = []
        for b in reads:
            self.readers.setdefault(b, []).append(ticket)
        self.last_ticket = ticket

    def op(self, eng, fn, reads=(), writes=(), samesync=True):
        pr = [k for k in reads if isinstance(k, str) and k.startswith("pb") and k not in writes]
        if pr:
            writes = list(writes) + pr
        self._need(eng, reads, writes, samesync)
        self.cnt[eng] += 1
        t = (eng, self.cnt[eng])
        self.prog[eng].append(("ins", fn, eng, 1))
        self._commit(t, reads, writes)
        return t

    def dma(self, q, out, in_, reads=(), writes=()):
        self._need(q, reads, writes, True)
        i = self.dma_n % self.n_dma_sems
        self.dma_n += 1
        sk = ("d", i)
        prev = 16 * self.dma_uses[i]
        if prev and self.waited[q].get(sk, 0) < prev:
            self.prog[q].append(("wait", sk, prev))
            self.waited[q][sk] = prev
        self.dma_uses[i] += 1
        t = (sk, 16 * self.dma_uses[i])
        self.prog[q].append(("ins", lambda e, o=out, s=in_: e.dma_start(out=o, in_=s), sk, 16))
        self._commit(t, reads, writes)
        return t

    def wait_ticket(self, eng, t):
        sk, v = t
        if self.waited[eng].get(sk, 0) < v:
            self.prog[eng].append(("wait", sk, v))
            self.waited[eng][sk] = v

    def barrier(self):
        for e in self.ENGS:
            for d in self.ENGS:
                if d != e and self.cnt[d]:
                    self.wait_ticket(e, (d, self.cnt[d]))
            for i in range(self.n_dma_sems):
                if self.dma_uses[i]:
                    self.wait_ticket(e, (("d", i), 16 * self.dma_uses[i]))

    def emit(self, stack):
        nc = self.nc
        for d in self.ENGS:
            if d != "sp" and self.cnt[d]:
                self.wait_ticket("sp", (d, self.cnt[d]))
        for i in range(self.n_dma_sems):
            if self.dma_uses[i]:
                self.wait_ticket("sp", (("d", i), 16 * self.dma_uses[i]))
        semh = {}
        for e in self.ENGS:
            semh[e] = stack.enter_context(nc.semaphore("s_" + e))
        for i in range(self.n_dma_sems):
            semh[("d", i)] = stack.enter_context(nc.semaphore("s_d%d" % i))
        block = stack.enter_context(nc.Block())

        def run(name):
            def body(e):
                for ent in self.prog[name]:
                    if ent[0] == "wait":
                        e.wait_ge(semh[ent[1]], ent[2])
                    else:
                        ent[1](e).then_inc(semh[ent[2]], ent[3])
            return body
        block.tensor(run("pe"))
        block.scalar(run("act"))
        block.vector(run("dve"))
        block.gpsimd(run("pool"))
        block.sync(run("sp"))


def build(dbg=None, stop_after=None, first_blk=0):
    dbg = dbg or {}
    nc = bass.Bass("TRN2", target_bir_lowering=False)
    S = Sched(nc)
    stack = contextlib.ExitStack()

    declared = []

    def din(name, shape, need=True):
        if not need:
            return None
        declared.append(name)
        return nc.dram_tensor(name, list(shape), F32, kind="ExternalInput").ap()
    moe_on = stop_after is None

    xw = din("xw", [NTOK, D])
    tmask = din("tmask", [128, NTOK])
    c_fm = din("c_fm", [128, KC])
    w_ada = din("w_ada", [D, 6 * D], "_modT" not in dbg)
    modT_in = din("modT_in", [128, 96], "_modT" in dbg)
    b_ada_fm = din("b_ada_fm", [128, 96])
    g1_fm = din("g1_fm", [128, KC])
    g2_fm = din("g2_fm", [128, KC])
    w_in = din("w_in", [D, 4640])
    mu_fm = din("mu_fm", [128, 28])
    w_dec = din("w_dec", [64, RW])
    w_aup = din("w_aup", [64, RW])
    w_gup = din("w_gup", [160, RW])
    w0_fm = din("w0_fm", [128, 8])
    a0_fm = din("a0_fm", [128, 8])
    kk_fm = din("kk_fm", [128, 8])
    ka_fm = din("ka_fm", [128, 8])
    rk_fm = din("rk_fm", [128, 8])
    lnw_fm = din("lnw_fm", [128, 8])
    lnb_fm = din("lnb_fm", [128, 8])
    sinks_bc = din("sinks_bc", [128, 16])
    w_o = din("w_o", [D, D])
    w_r = din("w_r", [D, 72])
    b_r_bc = din("b_r_bc", [128, 72])
    w1 = din("w1", [NE, D, FF], moe_on)
    w3 = din("w3", [NE, D, FF], moe_on)
    w2 = din("w2", [NE, FF, D], moe_on)
    fg_bc = din("fg_bc", [128, D])
    ident_d = din("ident", [128, 128])
    blk1_d = din("blk1", [128, 128])
    perm_d = din("perm", [128, 128])
    cosT_d = din("cosT", [128, 5 * NB])
    sinT_d = din("sinT", [128, 5 * NB])
    amask_d = din("amask", [128, 2, 256])
    mask1_d = din("mask1", [64, 2, 256])
    maskl_d = din("maskl", [64, 2, 64])
    ident2_d = din("ident2", [64, 2, 64])
    reset_d = din("resetm", [128, NB])
    out_d = nc.dram_tensor("out", [OWN, D], F32, kind="ExternalOutput").ap()
    dbg_out = {}
    for k, shp in dbg.items():
        if k.startswith("_"):
            continue
        dbg_out[k] = nc.dram_tensor("dbg_" + k, list(shp), F32, kind="ExternalOutput").ap()

    def sb(name, shape, dt=F32):
        return stack.enter_context(nc.sbuf_tensor(name, list(shape), dt))[:]

    def ps(name, shape, dt=F32):
        return stack.enter_context(nc.psum_tensor(name, list(shape), dt))[:]

    x1 = sb("x1", [128, 8, D])
    arena = sb("arena", [128, 31000])
    cst = sb("cst", [128, 3700])
    _c = [0]

    def carve(n, parts=128):
        a = cst[0:parts, _c[0]:_c[0] + n]
        _c[0] += n
        assert _c[0] <= 3700
        return a
    ident = carve(128); blk1 = carve(128); perm = carve(128)
    modT = carve(96); gs1 = carve(16); gs2 = carve(16); silc = carve(16); cfm = carve(16)
    g1t = carve(16); g2t = carve(16); bada = carve(96)
    mu = carve(28); w0t = carve(8); a0t = carve(8); kkt = carve(8); kat = carve(8); omka = carve(8)
    rkt = carve(8); lnw = carve(8); lnb = carve(8); sinks = carve(16); brbc = carve(72)
    amask = carve(512); mask1 = carve(512); maskl = carve(128); ident2 = carve(128)
    resetm = carve(NB); carry = carve(32)
    small = carve(256)
    ones128 = carve(128)
    PB = [ps("pb%d" % i, [128, 512]) for i in range(8)]

    def load_const(dst, src, key):
        S.dma("sp", dst, src, writes=[key])
    consts = [(ident, ident_d, "ident"), (blk1, blk1_d, "blk1"), (perm, perm_d, "perm"),
              (cfm, c_fm, "cfm"), (g1t, g1_fm, "g1t"), (g2t, g2_fm, "g2t"), (bada, b_ada_fm, "bada"),
              (mu, mu_fm, "mu"), (w0t, w0_fm, "w0t"), (a0t, a0_fm, "a0t"), (kkt, kk_fm, "kkt"),
              (kat, ka_fm, "kat"), (rkt, rk_fm, "rkt"), (lnw, lnw_fm, "lnw"), (lnb, lnb_fm, "lnb"),
              (sinks, sinks_bc, "sinks"), (brbc, b_r_bc, "brbc"),
              (amask, amask_d.rearrange("p a b -> p (a b)"), "amask"),
              (mask1[0:64, :], mask1_d.rearrange("p a b -> p (a b)"), "mask1"),
              (maskl[0:64, :], maskl_d.rearrange("p a b -> p (a b)"), "maskl"),
              (ident2[0:64, :], ident2_d.rearrange("p a b -> p (a b)"), "ident2"),
              (resetm, reset_d, "resetm")]
    for dst, src, key in consts:
        load_const(dst, src, key)
    S.op("pool", lambda e: e.memset(ones128, 1.0), writes=["ones128"])
    S.op("pool", lambda e: e.memset(carry, 0.0), writes=["carry%d" % g for g in range(32)])
    S.op("dve", lambda e: e.tensor_scalar(out=omka, in0=kat, scalar1=-1.0, scalar2=1.0,
                                          op0=ALU.mult, op1=ALU.add),
         reads=["kat"], writes=["omka"])

    def dump(name, src_ap, reads):
        if name in dbg_out:
            S.dma("sp", dbg_out[name], src_ap, reads=reads)

    class _Stop(Exception):
        pass

    def stop_here(tag):
        if stop_after == tag:
            raise _Stop()
    host_mod = "_modT" in dbg
    S.op("act", lambda e: e.activation(out=silc, in_=cfm, func=AF.Silu), reads=["cfm"], writes=["silc"])
    wada_v = w_ada.rearrange("(kc p) n -> p kc n", p=128) if w_ada is not None else None
    ADA_W = 512
    adabuf = [arena[:, i * KC * ADA_W:(i + 1) * KC * ADA_W].rearrange("p (k n) -> p k n", k=KC) for i in range(2)]
    pmod = PB[0][:, 0:96]
    if host_mod:
        S.dma("sp", modT, modT_in, writes=["modT"])
    for g in range(0 if host_mod else 6 * D // ADA_W):
        buf = adabuf[g % 2]
        bk = "adabuf%d" % (g % 2)
        S.dma("sp", buf, wada_v[:, :, g * ADA_W:(g + 1) * ADA_W], writes=[bk])
        for j in range(ADA_W // 128):
            cb = g * (ADA_W // 128) + j
            for kc in range(KC):
                S.op("pe", lambda e, cb=cb, kc=kc, j=j, buf=buf: e.matmul(
                    pmod[:, cb:cb + 1], lhsT=buf[:, kc, j * 128:(j + 1) * 128], rhs=silc[:, kc:kc + 1],
                    start=(kc == 0), stop=(kc == KC - 1)),
                    reads=[bk, "silc"], writes=["pmod"], samesync=False)
    if not host_mod:
        S.op("dve", lambda e: e.tensor_tensor(out=modT, in0=pmod, in1=bada, op=ALU.add),
             reads=["pmod", "bada"], writes=["modT"])
    S.op("dve", lambda e: e.scalar_tensor_tensor(out=gs1, in0=modT[:, 16:32], scalar=1.0, in1=g1t,
                                                 op0=ALU.add, op1=ALU.mult),
         reads=["modT", "g1t"], writes=["gs1"])
    S.op("dve", lambda e: e.scalar_tensor_tensor(out=gs2, in0=modT[:, 64:80], scalar=1.0, in1=g2t,
                                                 op0=ALU.add, op1=ALU.mult),
         reads=["modT", "g2t"], writes=["gs2"])
    dump("modT", modT, ["modT"])

    pe_cfg = [None]

    def pe_sync(stat):
        cfg = (stat.base_partition(), stat.partition_size(), stat.free_size())
        if pe_cfg[0] is not None and cfg != pe_cfg[0] and S.cnt["pe"]:
            S.wait_ticket("pe", ("pe", S.cnt["pe"]))
        pe_cfg[0] = cfg

    def mm(out, lhsT, rhs, start, stop, reads, writes):
        pe_sync(lhsT)
        S.op("pe", lambda e: e.matmul(out, lhsT=lhsT, rhs=rhs, start=start, stop=stop),
             reads=reads, writes=writes, samesync=False)

    def tr(out, in_, idn, reads, writes):
        pe_sync(in_)
        S.op("pe", lambda e: e.transpose(out, in_, idn), reads=list(reads) + ["ident"], writes=writes, samesync=False)

    def act(out, in_, func, reads, writes, **kw):
        S.op("act", lambda e: e.activation(out=out, in_=in_, func=func, **kw), reads=reads, writes=writes)

    def tt(eng, out, in0, in1, op, reads, writes):
        S.op(eng, lambda e: e.tensor_tensor(out=out, in0=in0, in1=in1, op=op), reads=reads, writes=writes)

    def ts(eng, out, in0, s1, s2, op0, op1, reads, writes):
        if s2 is None:
            S.op(eng, lambda e: e.tensor_scalar(out=out, in0=in0, scalar1=s1, scalar2=None, op0=op0),
                 reads=reads, writes=writes)
        else:
            S.op(eng, lambda e: e.tensor_scalar(out=out, in0=in0, scalar1=s1, scalar2=s2, op0=op0, op1=op1),
                 reads=reads, writes=writes)

    def stt(out, in0, scalar, in1, op0, op1, reads, writes):
        S.op("dve", lambda e: e.scalar_tensor_tensor(out=out, in0=in0, scalar=scalar, in1=in1, op0=op0, op1=op1),
             reads=reads, writes=writes)

    def cp(eng, out, in_, reads, writes):
        if eng == "act":
            S.op("act", lambda e: e.copy(out=out, in_=in_), reads=reads, writes=writes)
        else:
            S.op(eng, lambda e: e.tensor_copy(out=out, in_=in_), reads=reads, writes=writes)

    GT1 = sb("GT1", [128, D])
    bcs = arena[:, 0:128]

    def make_bcast(dst, col0, key, bcs=bcs, bk="bcs"):
        for kc in range(KC):
            ts("dve", bcs, ones128, modT[:, col0 + kc:col0 + kc + 1], None, ALU.mult, None,
               ["modT", "ones128"], [bk])
            tr(PB[1][:, 0:128], bcs, ident, [bk], ["pb1"])
            cp("act", dst[:, kc * 128:(kc + 1) * 128], PB[1][:, 0:128], ["pb1"], [key])
    make_bcast(GT1, 32, "GT1")
    dump("GT1", GT1, ["GT1"])
    if stop_after == "ada":
        S.emit(stack)
        return nc, stack, declared

    _a = [0]

    def acarve(n, parts=128):
        a = arena[0:parts, _a[0]:_a[0] + n]
        _a[0] += n
        assert _a[0] <= 31000, _a[0]
        return a
    for e in ("pe", "act", "dve", "pool", "sp"):
        S.wait_ticket(e, S.last_w["GT1"])
        S.wait_ticket(e, S.last_w["modT"])
    hT = acarve(KC * NB).rearrange("p (k n) -> p k n", k=KC)
    xtb = acarve(D)
    xn = acarve(D)
    wts = [acarve(KC * 128).rearrange("p (k n) -> p k n", k=KC) for _ in range(2)]
    Zt = [acarve(NB + 1) for _ in range(2)]
    ZSl = [acarve(NB) for _ in range(4)]
    ZSr = [[acarve(NB) for _ in range(3)] for _ in range(2)]
    tmt = acarve(NB)
    lwt = [acarve(4 * 128).rearrange("p (a n) -> p a n", a=4) for _ in range(2)]
    P_ = {n: acarve(NB) for n in ("sig", "cum", "a", "g", "t0", "t1", "kkn", "k2", "kna",
                                  "epos", "eneg", "eC", "bonus", "Bt", "Kt", "Bh", "Kh")}
    AR = acarve(2 * NB).rearrange("p (a n) -> p a n", a=2)
    TM = acarve(512, 64); M1 = acarve(512, 64).rearrange("p (h n) -> p h n", h=2)
    Lm = acarve(128, 64).rearrange("p (h n) -> p h n", h=2)
    XLb = [acarve(256, 64).rearrange("p (h a n) -> p h a n", h=2, a=2) for _ in range(2)]
    Pb = [acarve(128, 64).rearrange("p (h n) -> p h n", h=2) for _ in range(2)]
    AW = acarve(256, 64).rearrange("p (h n) -> p h n", h=2)
    AUs = acarve(256, 64).rearrange("p (h n) -> p h n", h=2)
    PhiT = acarve(128, 64).rearrange("p (h n) -> p h n", h=2)
    OmT = acarve(128, 64).rearrange("p (h n) -> p h n", h=2)
    Psis = acarve(128, 64).rearrange("p (h n) -> p h n", h=2)
    Dg = acarve(64)
    Mst = [acarve(16 * 64, 64).rearrange("p (h n) -> p h n", h=16) for _ in range(2)]
    Ysb = acarve(512, 64).rearrange("p (c h n) -> p c h n", c=4, h=2)
    gn = acarve(64, 64)
    oT = acarve(NB)
    wo = [acarve(512) for _ in range(2)]
    qraw = P_["Bt"]; qrot = P_["Kt"]; qtmp = P_["Bh"]
    kT = acarve(2 * 384).rearrange("p (h n) -> p h n", h=2)
    Vz = acarve(3 * 4 * 128).rearrange("p (t v n) -> p t v n", t=3, v=4)
    cst_ = P_["epos"]; snt_ = P_["eneg"]
    Ssb = P_["Kh"]; PTs = P_["kna"].rearrange("p (t n) -> p t n", t=2)

    S.op("pool", lambda e: e.memset(Mst[0], 0.0), writes=["M0_%d" % i for i in range(8)])
    S.op("pool", lambda e: e.memset(Vz, 0.0), writes=["Vz"])
    S.op("pool", lambda e: e.memset(kT, 0.0), writes=["kT"])
    w_in_v = w_in.rearrange("(kc p) n -> p kc n", p=128)
    sm_ss = small[:, 0:1]; sm_rt = small[:, 1:2]; sm_rstd = small[:, 2:3]
    RWB = ATT_COLS
    wt_i = [0]
    ps_i = [0]
    IN_SLOTS = [(PB[0][:, 0:256], "pb0"), (PB[1][:, 0:256], "pb1")]

    def inproj(col0, ncols, dup=False):
        wt = wts[wt_i[0] % 2]; wk = "wt%d" % (wt_i[0] % 2); wt_i[0] += 1
        if dup:
            S.dma("sp", wt[:, :, 0:64], w_in_v[:, :, col0:col0 + 64], writes=[wk])
            S.dma("sp", wt[:, :, 64:128], w_in_v[:, :, col0:col0 + 64], writes=[wk])
            ncols = 128
        else:
            S.dma("sp", wt[:, :, 0:ncols], w_in_v[:, :, col0:col0 + ncols], writes=[wk])
        pp, pk = IN_SLOTS[ps_i[0] % 2]; ps_i[0] += 1
        for kc in range(KC):
            mm(pp[0:ncols, :], wt[:, kc, 0:ncols], hT[:, kc, :], kc == 0, kc == KC - 1, [wk, "hT"], [pk])
        return pp[0:ncols, :], pk

    zt_i = [0]

    def tshift(pp, pk, ncols, g, dst, dkey):
        z = Zt[zt_i[0] % 2][0:ncols, :]; zk = "Zt%d" % (zt_i[0] % 2); zt_i[0] += 1
        cp("dve", z[:, 0:1], carry[0:ncols, g:g + 1], ["carry%d" % g], [zk])
        cp("act", z[:, 1:NB + 1], pp, [pk], [zk])
        tt("dve", dst[0:ncols, :], z[:, 0:NB], z[:, 1:NB + 1], ALU.subtract, [zk], [dkey])
        stt(dst[0:ncols, :], dst[0:ncols, :], mu[0:ncols, g:g + 1], z[:, 1:NB + 1], ALU.mult, ALU.add,
            [dkey, zk, "mu"], [dkey])
        cp("pool", carry[0:ncols, g:g + 1], z[:, NB:NB + 1], [zk], ["carry%d" % g])

    def outproj(wc, oTap, okey, blk):
        for cc in range(4):
            w = wo[cc % 2]; wk = "wo%d" % (cc % 2)
            c0 = cc * 512
            S.dma("sp", w, w_o[wc * 128:(wc + 1) * 128, c0:c0 + 512], writes=[wk])
            tt("pool", w, w, GT1[:, c0:c0 + 512], ALU.mult, [wk, "GT1"], [wk])
            for t2 in range(2):
                tile_i = (blk - FIRST_OWN_BLK) * 2 + t2
                mm(PB[2][:, :], oTap[:, t2 * 128:(t2 + 1) * 128], w, True, True, [okey, wk], ["pb2"])
                tt("dve", x1[:, tile_i, c0:c0 + 512], PB[2][:, :], x1[:, tile_i, c0:c0 + 512], ALU.add,
                   ["pb2", "x1_%d" % tile_i], ["x1_%d" % tile_i])

    GAM = -0.6065306597126334

    try:
      for blk in range(first_blk, NBLK):
          own = blk >= FIRST_OWN_BLK
          for t2 in range(2):
              t0 = blk * NB + t2 * 128
              if own:
                  tile_i = (blk - FIRST_OWN_BLK) * 2 + t2
                  xt = x1[:, tile_i, :]; xk = "x1_%d" % tile_i
              else:
                  xt = xtb; xk = "xtb"
              S.dma("sp", xt, xw[t0:t0 + 128, :], writes=[xk])
              act(xn, xt, AF.Square, [xk], ["xn", "ss"], accum_out=sm_ss)
              act(sm_rt, sm_ss, AF.Sqrt, ["ss"], ["rt"], scale=1.0 / D, bias=1e-6)
              S.op("dve", lambda e: e.reciprocal(out=sm_rstd, in_=sm_rt), reads=["rt"], writes=["rstd"])
              ts("dve", xn, xt, sm_rstd, None, ALU.mult, None, [xk, "rstd"], ["xn"])
              for g4 in range(4):
                  pbk = 2
                  for j in range(4):
                      kc = g4 * 4 + j
                      tr(PB[pbk][:, j * 128:(j + 1) * 128], xn[:, kc * 128:(kc + 1) * 128], ident, ["xn"],
                         ["pb%d" % pbk])
                  for j in range(4):
                      kc = g4 * 4 + j
                      act(hT[:, kc, t2 * 128:(t2 + 1) * 128], PB[pbk][:, j * 128:(j + 1) * 128], AF.Identity,
                          ["pb%d" % pbk, "gs1", "modT"], ["hT"],
                          scale=gs1[:, kc:kc + 1], bias=modT[:, kc:kc + 1])
          if not own:
              S.dma("sp", tmt, tmask[:, blk * NB:(blk + 1) * NB], writes=["tmt"])
              tt("dve", hT, hT, tmt.unsqueeze(1).to_broadcast([128, KC, NB]), ALU.mult, ["hT", "tmt"], ["hT"])
          if blk == FIRST_OWN_BLK:
              dump("hT12", hT.rearrange("p k n -> p (k n)"), ["hT"])
          stop_here("hT")

          if blk >= FIRST_OWN_BLK - 1:
              ab = blk - (FIRST_OWN_BLK - 1)
              S.dma("sp", cst_, cosT_d[:, ab * NB:(ab + 1) * NB], writes=["epos"])
              S.dma("sp", snt_, sinT_d[:, ab * NB:(ab + 1) * NB], writes=["eneg"])

              def rope(pp, pk, dst, dkey):
                  cp("act", qraw, pp, [pk], ["Bt"])
                  mm(PB[3][:, 0:256], perm, qraw, True, True, ["perm", "Bt"], ["pb3"])
                  tt("dve", qtmp, PB[3][:, 0:256], snt_, ALU.mult, ["pb3", "eneg"], ["Bh"])
                  tt("dve", dst, qraw, cst_, ALU.mult, ["Bt", "epos"], [dkey])
                  tt("dve", dst, dst, qtmp, ALU.add, [dkey, "Bh"], [dkey])
              for kvh in range(2):
                  pp, pk = inproj(1024 + kvh * 64, 64, dup=True)
                  rope(pp, pk, kT[:, kvh, 128:384], "kT")
              for t2 in range(2):
                  wt = wts[wt_i[0] % 2]; wk = "wt%d" % (wt_i[0] % 2); wt_i[0] += 1
                  if t2 == 0:
                      S.dma("sp", wt[:, :, 0:128], w_in_v[:, :, 1152:1280], writes=[wk])
                      wtv, wkv = wt, wk
                  else:
                      wt_i[0] -= 1
                  for kc in range(KC):
                      mm(PB[3][:, 256:384], hT[:, kc, t2 * 128:(t2 + 1) * 128], wtv[:, kc, 0:128], kc == 0, kc == KC - 1,
                         [wkv, "hT"], ["pb3"])
                  for kvh in range(2):
                      for par in range(2):
                          cp("act" if par else "dve", Vz[:, 1 + t2, kvh * 2 + par, par * 64:par * 64 + 64],
                             PB[3][:, 256 + kvh * 64:256 + kvh * 64 + 64], ["pb3"], ["Vz"])
              if own:
                  for hp in range(8):
                      kvh = hp // 4
                      pp, pk = inproj(hp * 128, 128)
                      rope(pp, pk, qrot, "Kt")
                      qM = (P_["sig"], P_["cum"])
                      ts("pool", qM[0], qrot, blk1[:, 0:1], None, ALU.mult, None, ["Kt", "blk1"], ["sig"])
                      ts("pool", qM[1], qrot, blk1[:, 64:65], None, ALU.mult, None, ["Kt", "blk1"], ["cum"])
                      for t2 in range(2):
                          qtile = (blk - FIRST_OWN_BLK) * 2 + t2
                          mi = 0 if qtile == 0 else 1
                          for par in range(2):
                              h = hp * 2 + par
                              b0 = par * 64
                              mm(PB[3][:, 256:512], qM[par][:, t2 * 128:(t2 + 1) * 128],
                                 kT[:, kvh, t2 * 128:t2 * 128 + 256], True, True, ["sig", "cum", "kT"], ["pb3"])
                              tt("dve", Ssb, PB[3][:, 256:512], amask[:, mi * 256:(mi + 1) * 256], ALU.add,
                                 ["pb3", "amask"], ["Kh"])
                              mx = small[:, 8:9]; nmx = small[:, 9:10]; rs = small[:, 10:11]; es = small[:, 11:12]
                              S.op("dve", lambda e, mx=mx: e.reduce_max(out=mx, in_=Ssb, axis=AX.X), reads=["Kh"], writes=["mx"])
                              ts("dve", nmx, mx, sinks[:, h:h + 1], -1.0, ALU.max, ALU.mult, ["mx", "sinks"], ["nmx"])
                              act(Ssb, Ssb, AF.Exp, ["Kh", "nmx"], ["Kh", "rs"], bias=nmx, accum_out=rs)
                              act(es, sinks[:, h:h + 1], AF.Exp, ["sinks", "nmx"], ["es"], bias=nmx)
                              tt("dve", rs, rs, es, ALU.add, ["rs", "es"], ["rs"])
                              S.op("dve", lambda e, rs=rs: e.reciprocal(out=rs, in_=rs), reads=["rs"], writes=["rs"])
                              ts("dve", Ssb, Ssb, rs, None, ALU.mult, None, ["Kh", "rs"], ["Kh"])
                              for kt in range(2):
                                  tr(PB[4][:, kt * 128:(kt + 1) * 128], Ssb[:, kt * 128:(kt + 1) * 128], ident, ["Kh"], ["pb4"])
                              cp("act", PTs.rearrange("p t n -> p (t n)"), PB[4][:, 0:256], ["pb4"], ["kna"])
                              for kt in range(2):
                                  mm(PB[5][:, t2 * 128:(t2 + 1) * 128], Vz[:, t2 + kt, kvh * 2 + par, :], PTs[:, kt, :],
                                     par == 0 and kt == 0, par == 1 and kt == 1, ["Vz", "kna"], ["pb5"])
                      cp("act", oT, PB[5][:, 0:256], ["pb5"], ["oT"])
                      if blk == FIRST_OWN_BLK and hp == 0:
                          dump("oTa", oT, ["oT"])
                      outproj(hp, oT, "oT", blk)
              cp("pool", kT[:, :, 0:128], kT[:, :, 256:384], ["kT"], ["kT"])
              cp("pool", Vz[:, 0, :, :], Vz[:, 2, :, :], ["Vz"], ["Vz"])
          stop_here("att")

          lora = [(RWB + 3072, 64, 24), (RWB + 3136, 64, 25), (RWB + 3200, 128, 26), (RWB + 3328, 32, 27)]
          for li, (c0, ncol, g) in enumerate(lora):
              pp, pk = inproj(c0, ncol)
              tshift(pp, pk, ncol, g, ZSl[li], "ZSl%d" % li)
          act(ZSl[0][0:64, :], ZSl[0][0:64, :], AF.Tanh, ["ZSl0"], ["ZSl0"])
          if own:
              act(ZSl[2], ZSl[2], AF.Sigmoid, ["ZSl2"], ["ZSl2"])
              act(ZSl[3][0:32, :], ZSl[3][0:32, :], AF.Sigmoid, ["ZSl3"], ["ZSl3"])
          stop_here("lora")
          for hp in range(8):
              zb = ZSr[hp % 2]; zbk = ["ZSr%d_%d" % (hp % 2, i) for i in range(3)]
              for i in range(3):
                  pp, pk = inproj(RWB + i * 1024 + hp * 128, 128)
                  tshift(pp, pk, 128, i * 8 + hp, zb[i], zbk[i])
              zr, zk_, zv = zb
              if blk == FIRST_OWN_BLK and hp == 0:
                  dump("zr", zr, [zbk[0]]); dump("zv", zv, [zbk[2]])
              lw = lwt[hp % 2]; lk = "lw%d" % (hp % 2)
              hs = slice(hp * 128, (hp + 1) * 128)
              S.dma("sp", lw[0:64, 0, :], w_dec[:, hs], writes=[lk])
              S.dma("sp", lw[0:64, 1, :], w_aup[:, hs], writes=[lk])
              if own:
                  S.dma("sp", lw[:, 2, :], w_gup[0:128, hs], writes=[lk])
                  S.dma("sp", lw[0:32, 3, :], w_gup[128:160, hs], writes=[lk])
              stop_here("rkv")
              p3a = PB[3][:, 0:256]; p3b = PB[3][:, 256:512]
              hcol = slice(hp, hp + 1)
              mm(p3a, lw[0:64, 0, :], ZSl[0][0:64, :], True, True, [lk, "ZSl0"], ["pb3"])
              act(P_["sig"], p3a, AF.Sigmoid, ["pb3", "w0t"], ["sig"], bias=w0t[:, hcol])
              ts("pool", P_["sig"], P_["sig"], GAM, None, ALU.mult, None, ["sig"], ["sig"])
              S.op("dve", lambda e: e.tensor_tensor_scan(out=P_["cum"], data0=resetm, data1=P_["sig"], initial=0.0,
                                                         op0=ALU.mult, op1=ALU.add),
                   reads=["sig", "resetm"], writes=["cum"])
              stop_here("decay")
              mm(p3b, lw[0:64, 1, :], ZSl[1][0:64, :], True, True, [lk, "ZSl1"], ["pb3"])
              act(P_["a"], p3b, AF.Sigmoid, ["pb3", "a0t"], ["a"], bias=a0t[:, hcol])
              ts("dve", P_["kkn"], zk_, kkt[:, hcol], None, ALU.mult, None, [zbk[1], "kkt"], ["kkn"])
              tt("pool", P_["t0"], P_["kkn"], P_["kkn"], ALU.mult, ["kkn"], ["t0"])
              mm(p3a, blk1, P_["t0"], True, True, ["blk1", "t0"], ["pb3"])
              act(P_["t1"], p3a, AF.Sqrt, ["pb3"], ["t1"], bias=1e-30)
              ts("dve", P_["t1"], P_["t1"], 1e-12, None, ALU.max, None, ["t1"], ["t1"])
              S.op("dve", lambda e: e.reciprocal(out=P_["t1"], in_=P_["t1"]), reads=["t1"], writes=["t1"])
              tt("dve", P_["kkn"], P_["kkn"], P_["t1"], ALU.mult, ["kkn", "t1"], ["kkn"])
              stop_here("kkn")
              ts("dve", P_["t0"], P_["a"], kat[:, hcol], omka[:, hcol], ALU.mult, ALU.add, ["a", "kat", "omka"], ["t0"])
              tt("dve", P_["k2"], zk_, P_["t0"], ALU.mult, [zbk[1], "t0"], ["k2"])
              tt("pool", P_["kna"], P_["kkn"], P_["a"], ALU.mult, ["kkn", "a"], ["kna"])
              stop_here("k2")
              act(P_["eneg"], P_["cum"], AF.Exp, ["cum"], ["eneg"], scale=-1.0)
              tt("dve", P_["t1"], P_["cum"], P_["sig"], ALU.subtract, ["cum", "sig"], ["t1"])
              act(P_["t1"], P_["t1"], AF.Exp, ["t1"], ["t1"])
              act(P_["epos"], P_["cum"], AF.Exp, ["cum"], ["epos"])
              for c in range(4):
                  cs_ = slice(c * 64, (c + 1) * 64)
                  act(P_["eC"][:, cs_], P_["cum"][:, cs_], AF.Exp, ["cum"], ["eC"], scale=-1.0,
                      bias=P_["cum"][:, c * 64 + 63:c * 64 + 64])
              stop_here("exps")
              stt(AR[:, 0, :], P_["kkn"], -1.0, P_["t1"], ALU.mult, ALU.mult, ["kkn", "t1"], ["AR"])
              tt("pool", P_["Bt"], P_["kna"], P_["eneg"], ALU.mult, ["kna", "eneg"], ["Bt"])
              tt("dve", P_["Kt"], P_["k2"], P_["eneg"], ALU.mult, ["k2", "eneg"], ["Kt"])
              tt("pool", P_["Bh"], P_["kna"], P_["eC"], ALU.mult, ["kna", "eC"], ["Bh"])
              tt("dve", P_["Kh"], P_["k2"], P_["eC"], ALU.mult, ["k2", "eC"], ["Kh"])
              stop_here("AR")
              if own:
                  tt("pool", AR[:, 1, :], zr, P_["epos"], ALU.mult, [zbk[0], "epos"], ["AR"])
                  mm(p3b, lw[:, 2, :], ZSl[2], True, False, [lk, "ZSl2"], ["pb3"])
                  mm(p3b, lw[0:32, 3, :], ZSl[3][0:32, :], False, True, [lk, "ZSl3"], ["pb3"])
                  cp("act", P_["g"], p3b, ["pb3"], ["g"])
                  stop_here("gate")
                  tt("dve", P_["t0"], zr, P_["k2"], ALU.mult, [zbk[0], "k2"], ["t0"])
                  ts("dve", P_["t0"], P_["t0"], rkt[:, hcol], None, ALU.mult, None, ["t0", "rkt"], ["t0"])
                  mm(p3a, blk1, P_["t0"], True, True, ["blk1", "t0"], ["pb3"])
                  tt("dve", P_["bonus"], p3a, zv, ALU.mult, ["pb3", zbk[2]], ["bonus"])
              BtM = (P_["sig"], P_["cum"]); KtM = (P_["a"], P_["t0"]); AtM = (P_["kkn"], P_["k2"])
              for h2 in range(2):
                  hmk = blk1[:, h2 * 64:h2 * 64 + 1]
                  ts("pool", BtM[h2], P_["Bt"], hmk, None, ALU.mult, None, ["Bt", "blk1"], [("sig", "cum")[h2]])
                  ts("dve", KtM[h2], P_["Kt"], hmk, None, ALU.mult, None, ["Kt", "blk1"], [("a", "t0")[h2]])
                  ts("pool", AtM[h2], AR[:, 0, :], hmk, None, ALU.mult, None, ["AR", "blk1"], [("kkn", "k2")[h2]])
              mm(PB[3][0:64, 0:4], ident[:, 64:128], P_["epos"].rearrange("p (c n) -> p c n", n=64)[:, :, 63],
                 True, True, ["ident", "epos"], ["pb3"])
              cp("act", Dg[0:64, 0:4], PB[3][0:64, 0:4], ["pb3"], ["Dg"])
              if own:
                  mm(PB[3][0:64, 256:512], ident[:, 64:128], AR[:, 1, :], True, True, ["ident", "AR"], ["pb3"])
                  cp("act", P_["t1"][0:64, :], PB[3][0:64, 256:512], ["pb3"], ["t1"])
              stop_here("prep")
              At = AR[:, 0, :]
              for c in range(4):
                  cs_ = slice(c * 64, (c + 1) * 64)
                  mcur = Mst[(blk * 4 + c) % 2]; mnew = Mst[(blk * 4 + c + 1) % 2]
                  mck = "M%d_%d" % ((blk * 4 + c) % 2, hp); mnk = "M%d_%d" % ((blk * 4 + c + 1) % 2, hp)
                  for i, (src, sk) in enumerate(((At, "AR"), (P_["Bh"], "Bh"), (P_["Kh"], "Kh"), (zv, zbk[2]))):
                      tr(PB[4][0:64, i * 128:(i + 1) * 128], src[:, cs_], ident, [sk], ["pb4"])
                  cp("act", TM[:, 128:512], PB[4][0:64, 128:512], ["pb4"], ["TM"])
                  cp("dve", AW[:, :, 0:64], PB[4][0:64, 0:128].rearrange("p (h n) -> p h n", h=2), ["pb4"], ["AW"])
                  stop_here("c_tm")
                  for h2 in range(2):
                      b0 = h2 * 64
                      nr = 2 if own else 1
                      mm(PB[5][0:64, h2 * 256:h2 * 256 + 64 * nr], BtM[h2][:, cs_], AR[:, 0:nr, cs_],
                         True, True, ["sig", "cum", "AR"], ["pb5"])
                      mm(PB[5][0:64, h2 * 256 + 128:h2 * 256 + 128 + 64 * nr], KtM[h2][:, cs_],
                         AR[:, 0:nr, cs_], True, True, ["a", "t0", "AR"], ["pb5"])
                      mm(PB[6][0:64, h2 * 64:(h2 + 1) * 64], AtM[h2][:, cs_], P_["Bt"][:, cs_], True, True,
                         ["kkn", "k2", "Bt"], ["pb6"])
                  if own:
                      tt("dve", M1.rearrange("p h n -> p (h n)"), PB[5][0:64, :], mask1[0:64, :], ALU.mult,
                         ["pb5", "mask1"], ["M1"])
                  else:
                      p5v = PB[5][0:64, :].rearrange("p (h n) -> p h n", h=2)
                      mkv = mask1[0:64, :].rearrange("p (h n) -> p h n", h=2)
                      for o_ in (0, 128):
                          tt("dve", M1[:, :, o_:o_ + 64], p5v[:, :, o_:o_ + 64], mkv[:, :, o_:o_ + 64], ALU.mult,
                             ["pb5", "mask1"], ["M1"])
                  tt("dve", Lm.rearrange("p h n -> p (h n)"), PB[6][0:64, 0:128], maskl[0:64, :], ALU.mult,
                     ["pb6", "maskl"], ["Lm"])
                  tt("pool", Pb[0], M1[:, :, 0:64], ident2[0:64, :].rearrange("p (h n) -> p h n", h=2), ALU.add,
                     ["M1", "ident2"], ["Pb0"])
                  stop_here("c_m1")
                  Xp = [M1[:, h2, 0:64] for h2 in range(2)]; Lp = [Lm[:, h2, :] for h2 in range(2)]
                  xk_, lk_ = ["M1"], ["Lm"]
                  pcur = 0
                  for m in range(5):
                      xl = XLb[m % 2]; xlk = "XL%d" % (m % 2)
                      for h2 in range(2):
                          mm(PB[6][0:64, 128 + h2 * 128:128 + h2 * 128 + 64], Lp[h2], Xp[h2], True, True, xk_ + lk_, ["pb6"])
                          mm(PB[6][0:64, 128 + h2 * 128 + 64:128 + h2 * 128 + 128], Xp[h2], Lp[h2], True, True, xk_ + lk_, ["pb6"])
                      cp("act", xl.rearrange("p h a n -> p (h a n)"), PB[6][0:64, 128:384], ["pb6"], [xlk])
                      Xp = [xl[:, h2, 0, :] for h2 in range(2)]; Lp = [xl[:, h2, 1, :] for h2 in range(2)]
                      xk_, lk_ = [xlk], [xlk]
                      for h2 in range(2):
                          mm(PB[6][0:64, 384 + h2 * 64:384 + (h2 + 1) * 64], Lp[h2], Pb[pcur][:, h2, :], True, True,
                             [xlk, "Pb%d" % pcur], ["pb6"])
                      tt("dve", Pb[1 - pcur].rearrange("p h n -> p (h n)"), PB[6][0:64, 384:512],
                         Pb[pcur].rearrange("p h n -> p (h n)"), ALU.add, ["pb6", "Pb%d" % pcur], ["Pb%d" % (1 - pcur)])
                      pcur = 1 - pcur
                  Pf = Pb[pcur]; pfk = "Pb%d" % pcur
                  stop_here("c_dbl")
                  for h2 in range(2):
                      mm(PB[6][0:64, h2 * 64:(h2 + 1) * 64], M1[:, h2, 128:192], TM[:, 384 + h2 * 64:384 + (h2 + 1) * 64],
                         True, True, ["M1", "TM"], ["pb6"])
                  cp("act", AW[:, :, 64:128], PB[6][0:64, 0:128].rearrange("p (h n) -> p h n", h=2), ["pb6"], ["AW"])
                  for h2 in range(2):
                      mm(PB[7][0:64, h2 * 128:(h2 + 1) * 128], Pf[:, h2, :], AW[:, h2, :], True, True, [pfk, "AW"], ["pb7"])
                  cp("act", AUs.rearrange("p h n -> p (h n)"), PB[7][0:64, 0:256], ["pb7"], ["AUs"])
                  stop_here("c_au")
                  for h2 in range(2):
                      b0 = h2 * 64
                      mm(PB[6][0:64, 128 + h2 * 64:128 + (h2 + 1) * 64], AUs[:, h2, 0:64], TM[:, 128 + b0:128 + b0 + 64],
                         True, True, ["AUs", "TM"], ["pb6"])
                      mm(PB[6][0:64, 256 + h2 * 64:256 + (h2 + 1) * 64], TM[:, 128 + b0:128 + b0 + 64], AUs[:, h2, 64:128],
                         True, False, ["AUs", "TM"], ["pb6"])
                      mm(PB[6][0:64, 256 + h2 * 64:256 + (h2 + 1) * 64], TM[:, 256 + b0:256 + b0 + 64],
                         TM[:, 384 + b0:384 + b0 + 64], False, True, ["TM"], ["pb6"])
                  stop_here("c_p1")
                  gcol = c * 64 + 63
                  stt(PhiT[:, 0, :], ident[0:64, 0:64], P_["epos"][0:64, gcol:gcol + 1], PB[6][0:64, 128:192],
                      ALU.mult, ALU.add, ["ident", "epos", "pb6"], ["PhiT"])
                  stt(PhiT[:, 1, :], ident[0:64, 0:64], Dg[0:64, c:c + 1], PB[6][0:64, 192:256],
                      ALU.mult, ALU.add, ["ident", "Dg", "pb6"], ["PhiT"])
                  cp("act", Psis.rearrange("p h n -> p (h n)"), PB[6][0:64, 256:384], ["pb6"], ["Psis"])
                  stop_here("c_phi")
                  if own:
                      for h2 in range(2):
                          mm(PB[6][0:64, 384 + h2 * 64:384 + (h2 + 1) * 64], AUs[:, h2, 0:64], M1[:, h2, 64:128],
                             True, True, ["AUs", "M1"], ["pb6"])
                      tt("dve", OmT[:, 0, :], PB[6][0:64, 384:448], AR[0:64, 1, cs_], ALU.add, ["pb6", "AR"], ["OmT"])
                      tt("dve", OmT[:, 1, :], PB[6][0:64, 448:512], P_["t1"][0:64, cs_], ALU.add, ["pb6", "t1"], ["OmT"])
                      for h2 in range(2):
                          hh = hp * 2 + h2
                          yo = PB[7][0:64, 256 + h2 * 64:256 + (h2 + 1) * 64]
                          mm(yo, OmT[:, h2, :], mcur[:, hh, :], True, False, ["OmT", mck], ["pb7"])
                          mm(yo, M1[:, h2, 64:128], AUs[:, h2, 64:128], False, False, ["M1", "AUs"], ["pb7"])
                          mm(yo, M1[:, h2, 192:256], TM[:, 384 + h2 * 64:384 + (h2 + 1) * 64], False, True, ["M1", "TM"], ["pb7"])
                      cp("act", Ysb[:, c, :, :], PB[7][0:64, 256:384].rearrange("p (h n) -> p h n", h=2), ["pb7"], ["Ysb"])
                  stop_here("c_y")
                  for h2 in range(2):
                      hh = hp * 2 + h2
                      mm(PB[7][0:64, 384 + h2 * 64:384 + (h2 + 1) * 64], PhiT[:, h2, :], mcur[:, hh, :], True, True,
                         ["PhiT", mck], ["pb7"])
                  tt("dve", mnew[:, hp * 2:hp * 2 + 2, :], PB[7][0:64, 384:512].rearrange("p (h n) -> p h n", h=2), Psis,
                     ALU.add, ["pb7", "Psis"], [mnk])
              stop_here("chunk")
              if own:
                  Yf = Ysb.rearrange("p c h n -> p (c h) n")
                  s1 = gn[:, 0:8]; s2 = gn[:, 8:16]
                  S.op("dve", lambda e: e.tensor_reduce(out=s1, in_=Yf, axis=AX.X, op=ALU.add), reads=["Ysb"], writes=["gn1"])
                  ts("dve", s1, s1, -1.0 / 64, None, ALU.mult, None, ["gn1"], ["gn1"])
                  tt("dve", Yf, Yf, s1.unsqueeze(2).to_broadcast([64, 8, 64]), ALU.add, ["Ysb", "gn1"], ["Ysb"])
                  ysq = TM[:, 0:512].rearrange("p (g n) -> p g n", g=8)
                  tt("dve", ysq, Yf, Yf, ALU.mult, ["Ysb"], ["TM"])
                  S.op("dve", lambda e: e.tensor_reduce(out=s2, in_=ysq, axis=AX.X, op=ALU.add), reads=["TM"], writes=["gn2"])
                  act(s2, s2, AF.Sqrt, ["gn2"], ["gn2"], scale=1.0 / 64, bias=64e-5)
                  S.op("dve", lambda e: e.reciprocal(out=s2, in_=s2), reads=["gn2"], writes=["gn2"])
                  tt("dve", Yf, Yf, s2.unsqueeze(2).to_broadcast([64, 8, 64]), ALU.mult, ["Ysb", "gn2"], ["Ysb"])
                  for c in range(4):
                      tr(PB[4][:, c * 64:(c + 1) * 64], Ysb[:, c, :, :].rearrange("p h n -> p (h n)"), ident[0:64, 0:64],
                         ["Ysb"], ["pb4"])
                  act(oT, PB[4][:, 0:256], AF.Identity, ["pb4", "lnw", "lnb"], ["oT"], scale=lnw[:, hcol], bias=lnb[:, hcol])
                  tt("dve", oT, oT, P_["bonus"], ALU.add, ["oT", "bonus"], ["oT"])
                  tt("dve", oT, oT, P_["g"], ALU.mult, ["oT", "g"], ["oT"])
                  if blk == FIRST_OWN_BLK and hp == 0:
                      dump("oTr", oT, ["oT"])
                  outproj(8 + hp, oT, "oT", blk)
    except _Stop:
        pass
    dump("x1", x1.rearrange("p t n -> p (t n)"), ["x1_%d" % i for i in range(8)])
    dump("Mst", Mst[0].rearrange("p h n -> p (h n)"), ["M0_%d" % i for i in range(8)])
    if stop_after is not None:
        S.emit(stack)
        return nc, stack, declared

    S.barrier()
    _a[0] = 0
    h2bf = acarve(KC * OWN // 2).bitcast(BF16).rearrange("p (k n) -> p k n", k=KC)
    h2f = acarve(KC * 128).rearrange("p (k n) -> p k n", k=KC)
    hidbf = acarve(4 * OWN // 2).bitcast(BF16).rearrange("p (f n) -> p f n", f=4)
    sil = [acarve(512) for _ in range(2)]
    w13 = [[acarve(KC * 128).rearrange("p (k n) -> p k n", k=KC) for _ in range(2)] for _ in range(2)]
    w13b = [[acarve(KC * 128 // 2).bitcast(BF16).rearrange("p (k n) -> p k n", k=KC) for _ in range(2)] for _ in range(2)]
    w2t = acarve(4 * 256).rearrange("p (f n) -> p f n", f=4)
    w2b = [acarve(4 * 256 // 2).bitcast(BF16).rearrange("p (f n) -> p f n", f=4) for _ in range(2)]
    Wt = acarve(8 * 64).rearrange("p (t e) -> p t e", t=8)
    rsc = acarve(256)
    bcs2 = acarve(128)
    xn2 = acarve(D)
    wr = w13[0][0].rearrange("p k n -> p (k n)")[:, 0:KC * 72].rearrange("p (k n) -> p k n", k=KC)
    make_bcast(GT1, 80, "GT1", bcs2, "bcs2")
    GT2 = GT1
    S.dma("sp", wr, w_r.rearrange("(kc p) n -> p kc n", p=128), writes=["w13_0_0"])
    for ti in range(8):
        xt = x1[:, ti, :]; xk = "x1_%d" % ti
        act(xn2, xt, AF.Square, [xk], ["xn2", "ss"], accum_out=sm_ss)
        act(sm_rt, sm_ss, AF.Sqrt, ["ss"], ["rt"], scale=1.0 / D, bias=1e-6)
        S.op("dve", lambda e: e.reciprocal(out=sm_rstd, in_=sm_rt), reads=["rt"], writes=["rstd"])
        ts("dve", xn2, xt, sm_rstd, None, ALU.mult, None, [xk, "rstd"], ["xn2"])
        for g4 in range(4):
            for j in range(4):
                kc = g4 * 4 + j
                tr(PB[1][:, j * 128:(j + 1) * 128], xn2[:, kc * 128:(kc + 1) * 128], ident, ["xn2"], ["pb1"])
            for j in range(4):
                kc = g4 * 4 + j
                act(h2f[:, kc, :], PB[1][:, j * 128:(j + 1) * 128], AF.Identity,
                    ["pb1", "gs2", "modT"], ["h2f"], scale=gs2[:, kc:kc + 1], bias=modT[:, 48 + kc:49 + kc])
        cp("pool", h2bf[:, :, ti * 128:(ti + 1) * 128], h2f, ["h2f"], ["h2T"])
        for kc in range(KC):
            mm(PB[3][:, 0:72], h2f[:, kc, :], wr[:, kc, :], kc == 0, kc == KC - 1,
               ["h2f", "w13_0_0"], ["pb3"])
        L = rsc[:, 0:72]; lg = rsc[:, 0:8]; le = rsc[:, 8:72]
        ohg = rsc[:, 72:80]; tmp64 = rsc[:, 80:144]; lsel = rsc[:, 144:152]; oh1 = rsc[:, 152:160]
        l2 = rsc[:, 160:168]; oh2 = rsc[:, 168:176]; we = rsc[:, 176:184]; eg = rsc[:, 184:192]
        mg = small[:, 16:17]; nmg = small[:, 17:18]; sg = small[:, 18:19]; m1 = small[:, 19:20]; m2 = small[:, 20:21]
        dd = small[:, 21:22]; e1 = small[:, 22:23]; e2 = small[:, 23:24]
        tt("dve", L, PB[3][:, 0:72], brbc, ALU.add, ["pb3", "brbc"], ["rsc"])
        S.op("dve", lambda e: e.reduce_max(out=mg, in_=lg, axis=AX.X), reads=["rsc"], writes=["rsm"])
        ts("dve", ohg, lg, mg, None, ALU.is_equal, None, ["rsc", "rsm"], ["rsc"])
        ts("dve", nmg, mg, -1.0, None, ALU.mult, None, ["rsm"], ["rsm"])
        act(eg, lg, AF.Exp, ["rsc", "rsm"], ["rsc", "rsm"], bias=nmg, accum_out=sg)
        S.op("dve", lambda e: e.reciprocal(out=sg, in_=sg), reads=["rsm"], writes=["rsm"])
        tt("dve", tmp64.rearrange("p (g e) -> p g e", g=8), le.rearrange("p (g e) -> p g e", g=8),
           ohg.unsqueeze(2).to_broadcast([128, 8, 8]), ALU.mult, ["rsc"], ["rsc"])
        S.op("dve", lambda e: e.tensor_reduce(out=lsel, in_=tmp64.rearrange("p (g e) -> p e g", g=8), axis=AX.X, op=ALU.add),
             reads=["rsc"], writes=["rsc"])
        S.op("dve", lambda e: e.reduce_max(out=m1, in_=lsel, axis=AX.X), reads=["rsc"], writes=["rsm"])
        ts("dve", oh1, lsel, m1, None, ALU.is_equal, None, ["rsc", "rsm"], ["rsc"])
        stt(l2, oh1, -1e30, lsel, ALU.mult, ALU.add, ["rsc"], ["rsc"])
        S.op("dve", lambda e: e.reduce_max(out=m2, in_=l2, axis=AX.X), reads=["rsc"], writes=["rsm"])
        ts("dve", oh2, l2, m2, None, ALU.is_equal, None, ["rsc", "rsm"], ["rsc"])
        tt("dve", dd, m1, m2, ALU.subtract, ["rsm"], ["rsm"])
        act(e1, dd, AF.Sigmoid, ["rsm"], ["rsm"])
        act(e2, dd, AF.Sigmoid, ["rsm"], ["rsm"], scale=-1.0)
        ts("dve", oh1, oh1, e1, None, ALU.mult, None, ["rsc", "rsm"], ["rsc"])
        stt(we, oh2, e2, oh1, ALU.mult, ALU.add, ["rsc", "rsm"], ["rsc"])
        ts("dve", we, we, sg, None, ALU.mult, None, ["rsc", "rsm"], ["rsc"])
        tt("dve", Wt[:, ti, :].rearrange("p (g e) -> p g e", g=8), ohg.unsqueeze(2).to_broadcast([128, 8, 8]),
           we.unsqueeze(1).to_broadcast([128, 8, 8]), ALU.mult, ["rsc"], ["Wt"])
    dump("Wt", Wt.rearrange("p t e -> p (t e)"), ["Wt"])
    n_exp = dbg.get("_n_exp", NE)
    wi = [0]
    si = [0]
    for ex in range(n_exp if moe_on else 0):
        for ffc in range(4):
            b = wi[0] % 2; wi[0] += 1
            w1c, w3c = w13[b]; k1, k3 = "w13_%d_0" % b, "w13_%d_1" % b
            w1b, w3b = w13b[b]; k1b, k3b = "w13b_%d_0" % b, "w13b_%d_1" % b
            S.dma("sp", w1c, w1[ex].rearrange("(kc p) n -> p kc n", p=128)[:, :, ffc * 128:(ffc + 1) * 128], writes=[k1])
            S.dma("sp", w3c, w3[ex].rearrange("(kc p) n -> p kc n", p=128)[:, :, ffc * 128:(ffc + 1) * 128], writes=[k3])
            cp("pool", w1b, w1c, [k1], [k1b])
            cp("act", w3b, w3c, [k3], [k3b])
            for th in range(2):
                tsl = slice(th * 512, (th + 1) * 512)
                for kc in range(KC):
                    mm(PB[0][:, :], w1b[:, kc, :], h2bf[:, kc, tsl], kc == 0, kc == KC - 1, [k1b, "h2T"], ["pb0"])
                for kc in range(KC):
                    mm(PB[1][:, :], w3b[:, kc, :], h2bf[:, kc, tsl], kc == 0, kc == KC - 1, [k3b, "h2T"], ["pb1"])
                sl_ = sil[si[0] % 2]; slk = "sil%d" % (si[0] % 2); si[0] += 1
                act(sl_, PB[0][:, :], AF.Silu, ["pb0"], [slk])
                tt("dve", hidbf[:, ffc, tsl], sl_, PB[1][:, :], ALU.mult, [slk, "pb1"], ["hid"])
        for cq in range(8):
            csl = slice(cq * 256, (cq + 1) * 256)
            wb = w2b[cq % 2]; wbk = "w2b%d" % (cq % 2)
            S.dma("sp", w2t, w2[ex].rearrange("(f p) n -> p f n", p=128)[:, :, csl], writes=["w2t"])
            tt("pool", wb, w2t, GT2[:, csl].unsqueeze(1).to_broadcast([128, 4, 256]), ALU.mult, ["w2t", "GT1"], [wbk])
            for ti in range(8):
                slot = (cq * 8 + ti) % 4
                pp = PB[2 + slot][:, 0:256]; pk = "pb%d" % (2 + slot)
                for ffc in range(4):
                    mm(pp, hidbf[:, ffc, ti * 128:(ti + 1) * 128], wb[:, ffc, :], ffc == 0, ffc == 3, ["hid", wbk], [pk])
                stt(x1[:, ti, csl], pp, Wt[:, ti, ex:ex + 1], x1[:, ti, csl], ALU.mult, ALU.add,
                    [pk, "Wt", "x1_%d" % ti], ["x1_%d" % ti])
    dump("x2", x1.rearrange("p t n -> p (t n)"), ["x1_%d" % i for i in range(8)])
    S.barrier()
    fg = w13[0][0].rearrange("p k n -> p (k n)")
    ob = [w13[0][1].rearrange("p k n -> p (k n)"), w13[1][0].rearrange("p k n -> p (k n)")]
    jk = w13[1][1].rearrange("p k n -> p (k n)")
    S.dma("sp", fg, fg_bc, writes=["fg"])
    outs = []
    for ti in range(8):
        xt = x1[:, ti, :]; xk = "x1_%d" % ti
        o = ob[ti % 2]; ok = "ob%d" % (ti % 2)
        act(jk, xt, AF.Square, [xk], ["jk", "ss"], accum_out=sm_ss)
        act(sm_rt, sm_ss, AF.Sqrt, ["ss"], ["rt"], scale=1.0 / D, bias=1e-6)
        S.op("dve", lambda e: e.reciprocal(out=sm_rstd, in_=sm_rt), reads=["rt"], writes=["rstd"])
        stt(o, xt, sm_rstd, fg, ALU.mult, ALU.mult, [xk, "rstd", "fg"], [ok])
        outs.append(S.dma("sp", out_d[ti * 128:(ti + 1) * 128, :], o, reads=[ok]))
    for t in outs:
        S.wait_ticket("sp", t)
    S.emit(stack)
    return nc, stack, declared


def _fm(v, n):
    return np.ascontiguousarray(np.asarray(v, np.float32).reshape(n, 128).T)


def _host_inputs(inp):
    f = np.float32
    x = np.asarray(inp["x"], f)
    c = np.asarray(inp["c"], f)
    w_in = np.ascontiguousarray(np.asarray(inp["w_in"], f)[0])
    shared = {}
    shared["w_ada"] = np.ascontiguousarray(np.asarray(inp["w_ada"], f)[0])
    shared["b_ada_fm"] = _fm(inp["b_ada"][0], 96)
    shared["g1_fm"] = _fm(inp["norm1_g"][0], 16)
    shared["g2_fm"] = _fm(inp["norm2_g"][0], 16)
    shared["w_in"] = w_in
    mu = np.asarray(inp["mu_shift"], f)[0]
    mu_fm = np.zeros((128, 28), f)
    for g in range(24):
        mu_fm[:, g] = mu[g * 128:(g + 1) * 128]
    mu_fm[0:64, 24] = mu[3072:3136]
    mu_fm[0:64, 25] = mu[3136:3200]
    mu_fm[:, 26] = mu[3200:3328]
    mu_fm[0:32, 27] = mu[3328:3360]
    shared["mu_fm"] = mu_fm
    shared["w_dec"] = np.ascontiguousarray(np.asarray(inp["w_decay_up"], f)[0])
    shared["w_aup"] = np.ascontiguousarray(np.asarray(inp["w_a_up"], f)[0])
    shared["w_gup"] = np.ascontiguousarray(np.asarray(inp["w_g_up"], f)[0])
    shared["w0_fm"] = _fm(inp["w0"][0], 8)
    shared["a0_fm"] = _fm(inp["a0"][0], 8)
    shared["kk_fm"] = _fm(inp["k_k"][0], 8)
    shared["ka_fm"] = _fm(inp["k_a"][0], 8)
    shared["rk_fm"] = _fm(np.asarray(inp["r_k"], f)[0].reshape(-1), 8)
    shared["lnw_fm"] = _fm(inp["ln_x_w"][0], 8)
    shared["lnb_fm"] = _fm(inp["ln_x_b"][0], 8)
    shared["sinks_bc"] = np.ascontiguousarray(np.broadcast_to(np.asarray(inp["sinks"], f)[0][None, :], (128, 16)))
    shared["w_o"] = np.ascontiguousarray(np.asarray(inp["w_o"], f)[0])
    shared["w_r"] = np.ascontiguousarray(np.concatenate(
        [np.asarray(inp["w_router_group"], f)[0], np.asarray(inp["w_router_expert"], f)[0]], axis=1))
    b_r = np.concatenate([np.asarray(inp["b_router_group"], f)[0], np.asarray(inp["b_router_expert"], f)[0]])
    shared["b_r_bc"] = np.ascontiguousarray(np.broadcast_to(b_r[None, :], (128, 72)))
    shared["w1"] = np.asarray(inp["w1"], f)[0]
    shared["w3"] = np.asarray(inp["w3"], f)[0]
    shared["w2"] = np.asarray(inp["w2"], f)[0]
    shared["fg_bc"] = np.ascontiguousarray(np.broadcast_to(np.asarray(inp["final_g"], f)[None, :], (128, D)))
    shared["ident"] = np.eye(128, dtype=f)
    b1 = np.zeros((128, 128), f); b1[:64, :64] = 1; b1[64:, 64:] = 1
    shared["blk1"] = b1
    pm = np.zeros((128, 128), f)
    for m in range(128):
        base = (m // 64) * 64; i = m % 64
        pm[base + (i + 32) % 64, m] = 1
    shared["perm"] = pm
    up_s = np.triu(np.ones((64, 64), f), 1); up_i = np.triu(np.ones((64, 64), f), 0)
    m1 = np.concatenate([up_s, up_i, up_s, up_i], axis=1)
    shared["mask1"] = np.ascontiguousarray(np.stack([m1, m1], axis=1))
    lo_s = np.tril(np.ones((64, 64), f), -1)
    shared["maskl"] = np.ascontiguousarray(np.stack([lo_s, lo_s], axis=1))
    shared["ident2"] = np.ascontiguousarray(np.stack([np.eye(64, dtype=f)] * 2, axis=1))
    rm = np.ones((128, NB), f); rm[:, ::64] = 0
    shared["resetm"] = rm
    inv_freq = (10000.0 ** (-np.arange(0, 64, 2, dtype=f) / f(64))).astype(f)
    qpos = np.arange(128)[:, None]; kpos = np.arange(256)[None, :] - 128
    diff = qpos - kpos
    band = (diff >= 0) & (diff < 128)
    per_core = []
    for core in range(8):
        b, q = core // 4, core % 4
        m = dict(shared)
        start = 1024 * (q + 1) - NTOK
        win = np.zeros((NTOK, D), f)
        lo = max(start, 0)
        win[lo - start:] = x[b, lo:1024 * (q + 1)]
        m["xw"] = win
        tm = np.zeros((128, NTOK), f); tm[:, lo - start:] = 1
        m["tmask"] = tm
        m["c_fm"] = _fm(c[b], 16)
        pos = (np.arange(5 * NB) + (1024 * q - NB)).astype(f)
        ang = pos[None, :] * np.tile(inv_freq, 4)[:, None]
        cs = np.cos(ang).astype(f) * f(0.125 ** 0.5)
        sn = np.sin(ang).astype(f) * f(0.125 ** 0.5)
        sign = np.where((np.arange(128) % 64) < 32, -1.0, 1.0).astype(f)[:, None]
        m["cosT"] = np.ascontiguousarray(cs)
        m["sinT"] = np.ascontiguousarray(sn * sign)
        am = np.where(band, 0.0, -30000.0).astype(f)
        am0 = am.copy()
        if q == 0:
            am0[:, :128] = -30000.0
        m["amask"] = np.ascontiguousarray(np.stack([am0, am], axis=1))
        per_core.append(m)
    return per_core


def kernel(**inputs):
    nc, stack, declared = build()
    with stack:
        pass
    in_maps = [{k: m[k] for k in declared} for m in _host_inputs(inputs)]
    res = run_bass_kernel_spmd(nc, in_maps, core_ids=list(range(8)))
    out = np.zeros((2, 4096, D), np.float32)
    for core in range(8):
        b, q = core // 4, core % 4
        out[b, 1024 * q:1024 * (q + 1)] = res.results[core]["out"]
    return out
```
